# Optimizing a Trainium2 kernel written in Bass

```python
import jax, jax.numpy as jnp
from jax import lax
import numpy as np

D_MODEL = 1024
BATCH = 8
SEQ = 4096
DEPTH = 1

HEAD_DIM = 64
N_HEADS_DSA = (D_MODEL // 2) // HEAD_DIM
N_HEADS_MOBA = (D_MODEL // 2) // HEAD_DIM
W_DSA = N_HEADS_DSA * HEAD_DIM
W_MOBA = N_HEADS_MOBA * HEAD_DIM
N_IDX_HEADS = 4
IDX_DIM = 64
DSA_TOPK = 256
MOBA_BLOCK = 256
MOBA_TOPK = 3
D_FF = ((8 * D_MODEL // 3 + 255) // 256) * 256
D_PLE = 256
ROPE_THETA = 10000.0
EPS = 1e-6
DSA_Q_BLOCK = 128
MOBA_Q_BLOCK = 16
IN_SPLITS = (W_DSA, HEAD_DIM, HEAD_DIM, N_IDX_HEADS * IDX_DIM, IDX_DIM, N_IDX_HEADS,
             W_MOBA, W_MOBA, W_MOBA)
N_IN = sum(IN_SPLITS)

kernel_name = "hymba_dsa_moba_hybrid_layer"


def rmsnorm(x, g):
    xf = x.astype(jnp.float32)
    y = xf * lax.rsqrt(jnp.mean(xf * xf, axis=-1, keepdims=True) + EPS)
    return (y * g.astype(jnp.float32)).astype(x.dtype)


def layernorm(x, g):
    xf = x.astype(jnp.float32)
    mu = jnp.mean(xf, axis=-1, keepdims=True)
    var = jnp.mean(jnp.square(xf - mu), axis=-1, keepdims=True)
    return ((xf - mu) * lax.rsqrt(var + EPS) * g.astype(jnp.float32)).astype(x.dtype)


def rope(x, pos):
    half = x.shape[-1] // 2
    inv = ROPE_THETA ** (-jnp.arange(half, dtype=jnp.float32) / half)
    ang = pos.astype(jnp.float32)[:, None] * inv[None, :]
    cos = jnp.cos(ang)[:, None, :]
    sin = jnp.sin(ang)[:, None, :]
    x1 = x[..., :half].astype(jnp.float32)
    x2 = x[..., half:].astype(jnp.float32)
    return jnp.concatenate([x1 * cos - x2 * sin, x2 * cos + x1 * sin], axis=-1).astype(x.dtype)


def dsa_attention(q, k, v, q_idx, k_idx, w_idx):
    B, L, H, D = q.shape
    top = min(DSA_TOPK, L // 4)
    n_blocks = L // DSA_Q_BLOCK
    key_pos = jnp.arange(L)
    b_ix = jnp.arange(B)[:, None, None]
    w_s = w_idx.astype(jnp.float32) * (N_IDX_HEADS ** -0.5) * (IDX_DIM ** -0.5)
    scale = D ** -0.5

    def one_block(c):
        t0 = c * DSA_Q_BLOCK
        qb = lax.dynamic_slice_in_dim(q, t0, DSA_Q_BLOCK, axis=1)
        qib = lax.dynamic_slice_in_dim(q_idx, t0, DSA_Q_BLOCK, axis=1)
        wb = lax.dynamic_slice_in_dim(w_s, t0, DSA_Q_BLOCK, axis=1)
        qpos = t0 + jnp.arange(DSA_Q_BLOCK)
        causal = key_pos[None, :] <= qpos[:, None]
        rel = jax.nn.relu(jnp.einsum('bthd,bsd->bths', qib, k_idx).astype(jnp.float32))
        score = jnp.einsum('bth,bths->bts', wb, rel)
        score = jnp.where(causal[None], score, -jnp.inf)
        _, idx = lax.top_k(score, top)
        ks = k[b_ix, idx]
        vs = v[b_ix, idx]
        valid = idx <= qpos[None, :, None]
        logits = jnp.einsum('bthd,btkd->bthk', qb, ks).astype(jnp.float32) * scale
        logits = jnp.where(valid[:, :, None, :], logits, -jnp.inf)
        prob = jax.nn.softmax(logits, axis=-1).astype(v.dtype)
        return jnp.einsum('bthk,btkd->bthd', prob, vs)

    out = lax.map(one_block, jnp.arange(n_blocks))
    return out.transpose(1, 0, 2, 3, 4).reshape(B, L, H, D)


def moba_attention(q, k, v):
    B, L, H, D = q.shape
    nb = -(-L // MOBA_BLOCK)
    pad = nb * MOBA_BLOCK - L
    kp = jnp.pad(k, ((0, 0), (0, pad), (0, 0), (0, 0)))
    vp = jnp.pad(v, ((0, 0), (0, pad), (0, 0), (0, 0)))
    k_blk = kp.reshape(B, nb, MOBA_BLOCK, H, D).transpose(0, 3, 1, 2, 4)
    v_blk = vp.reshape(B, nb, MOBA_BLOCK, H, D).transpose(0, 3, 1, 2, 4)
    k_mean = jnp.mean(k_blk.astype(jnp.float32), axis=3)
    n_sel = min(MOBA_TOPK, nb - 1)
    b_ix = jnp.arange(B)[:, None, None, None]
    h_ix = jnp.arange(H)[None, None, :, None]
    blk_ids = jnp.arange(nb)
    own_off = jnp.arange(MOBA_BLOCK)
    scale = D ** -0.5
    n_chunks = L // MOBA_Q_BLOCK

    def one_chunk(c):
        t0 = c * MOBA_Q_BLOCK
        j = t0 // MOBA_BLOCK
        qb = lax.dynamic_slice_in_dim(q, t0, MOBA_Q_BLOCK, axis=1)
        qpos = t0 + jnp.arange(MOBA_Q_BLOCK)
        k_own = lax.dynamic_slice_in_dim(kp, j * MOBA_BLOCK, MOBA_BLOCK, axis=1)
        v_own = lax.dynamic_slice_in_dim(vp, j * MOBA_BLOCK, MOBA_BLOCK, axis=1)
        own_mask = (j * MOBA_BLOCK + own_off)[None, :] <= qpos[:, None]
        l_own = jnp.einsum('bthd,bshd->bths', qb, k_own).astype(jnp.float32) * scale
        l_own = jnp.where(own_mask[None, :, None, :], l_own, -jnp.inf)
        if n_sel == 0:
            prob = jax.nn.softmax(l_own, axis=-1).astype(v.dtype)
            return jnp.einsum('bths,bshd->bthd', prob, v_own)
        gate = jnp.einsum('bthd,bhnd->bthn', qb.astype(jnp.float32), k_mean)
        gate = jnp.where(blk_ids < j, gate, -jnp.inf)
        _, sel = lax.top_k(gate, n_sel)
        valid = sel < j
        k_sel = k_blk[b_ix, h_ix, sel]
        v_sel = v_blk[b_ix, h_ix, sel]
        l_past = jnp.einsum('bthd,bthnsd->bthns', qb, k_sel).astype(jnp.float32) * scale
        l_past = jnp.where(valid[..., None], l_past, -jnp.inf)
        l_past = l_past.reshape(B, MOBA_Q_BLOCK, H, n_sel * MOBA_BLOCK)
        prob = jax.nn.softmax(jnp.concatenate([l_past, l_own], axis=-1), axis=-1).astype(v.dtype)
        p_past = prob[..., :n_sel * MOBA_BLOCK]
        p_own = prob[..., n_sel * MOBA_BLOCK:]
        v_sel = v_sel.reshape(B, MOBA_Q_BLOCK, H, n_sel * MOBA_BLOCK, D)
        return (jnp.einsum('bthk,bthkd->bthd', p_past, v_sel)
                + jnp.einsum('bths,bshd->bthd', p_own, v_own))

    out = lax.map(one_chunk, jnp.arange(n_chunks))
    return out.transpose(1, 0, 2, 3, 4).reshape(B, L, H, D)


def setup_inputs(seed: int = 0) -> dict:
    key = jax.random.key(seed)
    ks = jax.random.split(key, 16)
    f32 = jnp.float32
    nrm = lambda k, shape, fan_in: jax.random.normal(k, shape, f32) * (fan_in ** -0.5)
    gain = lambda k, n: 1.0 + 0.02 * jax.random.normal(k, (DEPTH, n), f32)
    return {
        "x": jax.random.normal(ks[0], (BATCH, SEQ, D_MODEL), f32),
        "p": jax.random.normal(ks[1], (DEPTH, BATCH, SEQ, D_PLE), f32),
        "g_attn": gain(ks[2], D_MODEL),
        "w_in": nrm(ks[3], (DEPTH, D_MODEL, N_IN), D_MODEL),
        "g_idx_k": gain(ks[4], IDX_DIM),
        "w_out": nrm(ks[5], (DEPTH, W_DSA + W_MOBA, D_MODEL), W_DSA + W_MOBA),
        "g_ffn": gain(ks[6], D_MODEL),
        "w_gate": nrm(ks[7], (DEPTH, D_MODEL, D_FF), D_MODEL),
        "w_up": nrm(ks[8], (DEPTH, D_MODEL, D_FF), D_MODEL),
        "w_down": nrm(ks[9], (DEPTH, D_FF, D_MODEL), D_FF),
        "g_ple": gain(ks[10], D_MODEL),
        "w_ple_gate": nrm(ks[11], (DEPTH, D_MODEL, D_MODEL), D_MODEL),
        "w_ple_proj": nrm(ks[12], (DEPTH, D_PLE, D_MODEL), D_PLE),
        "g_final": 1.0 + 0.02 * jax.random.normal(ks[13], (D_MODEL,), f32),
    }


def reference(x, p, g_attn, w_in, g_idx_k, w_out, g_ffn, w_gate, w_up, w_down,
              g_ple, w_ple_gate, w_ple_proj, g_final):
    B, L, _ = x.shape
    pos = jnp.arange(L)
    offs = [int(o) for o in np.cumsum(IN_SPLITS)[:-1]]
    h = x
    for i in range(DEPTH):
        a = rmsnorm(h, g_attn[i])
        proj = a @ w_in[i]
        qa, ka, va, qi, ki, wi, qb, kb, vb = jnp.split(proj, offs, axis=-1)
        qa = rope(qa.reshape(B, L, N_HEADS_DSA, HEAD_DIM), pos)
        ka = rope(ka.reshape(B, L, 1, HEAD_DIM), pos)[:, :, 0]
        qi = rope(qi.reshape(B, L, N_IDX_HEADS, IDX_DIM), pos)
        ki = rope(layernorm(ki, g_idx_k[i]).reshape(B, L, 1, IDX_DIM), pos)[:, :, 0]
        qb = rope(qb.reshape(B, L, N_HEADS_MOBA, HEAD_DIM), pos)
        kb = rope(kb.reshape(B, L, N_HEADS_MOBA, HEAD_DIM), pos)
        vb = vb.reshape(B, L, N_HEADS_MOBA, HEAD_DIM)
        y_a = dsa_attention(qa, ka, va, qi, ki, wi).reshape(B, L, W_DSA)
        y_b = moba_attention(qb, kb, vb).reshape(B, L, W_MOBA)
        h = h + jnp.concatenate([y_a, y_b], axis=-1) @ w_out[i]
        f = rmsnorm(h, g_ffn[i])
        h = h + (jax.nn.silu(f @ w_gate[i]) * (f @ w_up[i])) @ w_down[i]
        u = rmsnorm(h, g_ple[i])
        h = h + jax.nn.sigmoid(u @ w_ple_gate[i]) * (p[i] @ w_ple_proj[i])
    return rmsnorm(h, g_final)
```

```python
import numpy as np
import concourse.bass as bass
import concourse.mybir as mybir
from concourse.bass_utils import run_bass_kernel_spmd

F32 = mybir.dt.float32
BF16 = mybir.dt.bfloat16
ALU = mybir.AluOpType
AF = mybir.ActivationFunctionType
AX = mybir.AxisListType

D = 1024
NIN = 2500
DFF = 2816
NFC = 22
DPLE = 256
C_QA, C_KA, C_VA, C_QI, C_KI, C_WI, C_QB, C_KB, C_VB = 0, 512, 576, 640, 896, 960, 964, 1476, 1988
S_QA, S_KA, S_QI, S_QB, S_KB, S_SG, QW = 0, 512, 576, 832, 1344, 1856, 1860
NEG = -30000.0
BIG = 1.0e30
KIT = 16
EPS = 1e-6
COMPUTE = ("pe", "act", "dve", "pool")
import os
WARMUP = int(os.environ.get("K_WARMUP", "20"))


class Op:
    __slots__ = ("idx", "eng", "fn", "deps", "dma", "chan", "flag", "cnt", "semkey", "tag")

    def __init__(self, idx, eng, fn, deps, dma, chan):
        self.idx = idx
        self.eng = eng
        self.fn = fn
        self.deps = deps
        self.dma = dma
        self.chan = chan
        self.flag = False
        self.cnt = 0
        self.semkey = None


class Prog:
    def __init__(self):
        self.ops = []
        self.last_writer = {}
        self.readers = {}
        self.last_eng = {}
        self.last_chan = {}
        self.tag = ""
        self.names = {}

    def add(self, eng, fn, reads=(), writes=(), dma=False, chan=None, extra=()):
        idx = len(self.ops)
        deps = set(extra)
        for r in reads:
            w = self.last_writer.get(r)
            if w is not None:
                deps.add(w)
            if isinstance(r, tuple) and r[0] == "b":
                for rd in self.readers.get(r, ()):
                    if self.ops[rd].eng != eng:
                        deps.add(rd)
        for r in writes:
            w = self.last_writer.get(r)
            if w is not None:
                deps.add(w)
            for rd in self.readers.get(r, ()):
                deps.add(rd)
        for r in reads:
            self.readers.setdefault(r, []).append(idx)
        for r in writes:
            self.last_writer[r] = idx
            self.readers[r] = []
        deps.discard(idx)
        self.ops.append(Op(idx, eng, fn, deps, dma, chan))
        self.ops[-1].tag = self.tag
        if fn is not None:
            if dma:
                self.last_chan[chan] = idx
            else:
                self.last_eng[eng] = idx
        return idx

    def pe(self, fn, reads=(), writes=()):
        return self.add("pe", fn, reads, writes)

    def act(self, fn, reads=(), writes=()):
        return self.add("act", fn, reads, writes)

    def dve(self, fn, reads=(), writes=()):
        return self.add("dve", fn, reads, writes)

    def pool(self, fn, reads=(), writes=()):
        return self.add("pool", fn, reads, writes)

    def dma(self, eng, chan, fn, reads=(), writes=()):
        return self.add(eng, fn, reads, writes, dma=True, chan=chan)

    def barrier(self):
        deps = set(self.last_eng.values()) | set(self.last_chan.values())
        for e in ("sp", "pe", "act", "dve", "pool"):
            self.add(e, None, extra=deps)
        self.last_writer = {}
        self.readers = {}

    def emit(self, nc, final_wait_eng="sp"):
        ops = self.ops
        for op in ops:
            last = {}
            for d in op.deps:
                p = ops[d]
                if p.dma:
                    p.flag = True
                elif op.dma or p.eng != op.eng or p.eng in ("act", "dve", "pool"):
                    if last.get(p.eng, -1) < d:
                        last[p.eng] = d
            for d in last.values():
                ops[d].flag = True
        chans = {}
        engs = {}
        for op in ops:
            if op.fn is None:
                continue
            if op.dma:
                op.flag = True
                c = chans.get(op.chan, 0) + 16
                chans[op.chan] = c
                op.cnt = c
                op.semkey = ("dma", op.chan)
            else:
                if op.flag:
                    c = engs.get(op.eng, 0) + 1
                    engs[op.eng] = c
                    op.cnt = c
                op.semkey = ("eng", op.eng)
        semkeys = [("eng", e) for e in COMPUTE] + [("dma", c) for c in chans]
        self.nsems = len(semkeys)
        streams = {}
        for op in ops:
            streams.setdefault(op.eng, []).append(op)
        sems = {}
        for k in semkeys:
            sems[k] = nc.alloc_semaphore(name=("s_%s_%s" % k))
        attr = {"pe": "tensor", "act": "scalar", "dve": "vector", "pool": "gpsimd", "sp": "sync"}
        with nc.Block() as block0:

            def clear_body(eng):
                for k in semkeys:
                    eng.sem_clear(sems[k])

            block0.sync(clear_body)
        with nc.Block() as block:

            def make_body(elist, is_final):
                def body(eng):
                    waited = {}
                    for op in elist:
                        need = {}
                        lastd = {}
                        for d in op.deps:
                            p = ops[d]
                            if p.dma:
                                lastd[("c", d)] = d
                            elif lastd.get(p.eng, -1) < d:
                                lastd[p.eng] = d
                        for d in lastd.values():
                            p = ops[d]
                            if not p.flag:
                                continue
                            if (not p.dma) and (not op.dma) and p.eng == op.eng and op.eng == "pe":
                                continue
                            k = p.semkey
                            if need.get(k, 0) < p.cnt:
                                need[k] = p.cnt
                        for k, v in need.items():
                            if waited.get(k, 0) >= v:
                                continue
                            eng.wait_ge(sems[k], v)
                            waited[k] = v
                        if op.fn is None:
                            continue
                        ins = op.fn(eng)
                        try:
                            self.names[ins.ins.name] = op.tag
                        except Exception:
                            pass
                        if op.flag:
                            ins.then_inc(sems[op.semkey], 16 if op.dma else 1)
                    if is_final:
                        for c, v in chans.items():
                            if waited.get(("dma", c), 0) < v:
                                eng.wait_ge(sems[("dma", c)], v)
                        for e, v in engs.items():
                            if v > 0 and waited.get(("eng", e), 0) < v:
                                eng.wait_ge(sems[("eng", e)], v)

                return body

            for ename in ("sp", "pe", "act", "dve", "pool"):
                elist = streams.get(ename, [])
                is_final = ename == final_wait_eng
                if not elist and not is_final:
                    continue
                getattr(block, attr[ename])(make_body(elist, is_final))
        return nc


class SB:
    def __init__(self, nc, base=16512, top=229344):
        self.nc = nc
        self.off = base
        self.top = top
        self.n = 0
        self.peak = base

    def alloc(self, name, shape, dt):
        esz = 4 if dt == F32 else 2
        nb = esz
        for s in shape[1:]:
            nb *= s
        self.n += 1
        t = self.nc.alloc_sbuf_tensor_at("%s_%d" % (name, self.n), list(shape), dt, offset=self.off)
        self.off += (nb + 31) // 32 * 32
        assert self.off <= self.top, "SBUF overflow at %s: %d > %d" % (name, self.off, self.top)
        self.peak = max(self.peak, self.off)
        return t

    def mark(self):
        return self.off

    def reset(self, m):
        self.off = m


class KB:
    def __init__(self, P):
        self.P = P

    def mm(self, out, lhsT, rhs, start, stop, reads, writes, skip=False):
        self.P.pe(lambda e: e.matmul(out, lhsT=lhsT, rhs=rhs, start=start, stop=stop, skip_group_check=skip), reads, writes)

    def tr(self, out, in_, ident, reads, writes):
        self.P.pe(lambda e: e.transpose(out, in_, ident), reads, writes)

    def actf(self, out, in_, func, reads, writes, scale=None, bias=None, accum=None):
        kw = {}
        if scale is not None:
            kw["scale"] = scale
        if bias is not None:
            kw["bias"] = bias
        if accum is not None:
            kw["accum_out"] = accum
        self.P.act(lambda e: e.activation(out=out, in_=in_, func=func, **kw), reads, writes)

    def ts(self, eng, out, in0, s1, s2, op0, op1, reads, writes, accum=None):
        kw = {}
        if op1 is not None:
            kw["op1"] = op1
        if accum is not None:
            kw["accum_out"] = accum
        self.P.add(eng, lambda e: e.tensor_scalar(out=out, in0=in0, scalar1=s1, scalar2=s2, op0=op0, **kw), reads, writes)

    def tt(self, eng, out, in0, in1, op, reads, writes):
        self.P.add(eng, lambda e: e.tensor_tensor(out=out, in0=in0, in1=in1, op=op), reads, writes)

    def stt(self, out, in0, scalar, in1, op0, op1, reads, writes):
        self.P.dve(lambda e: e.scalar_tensor_tensor(out=out, in0=in0, scalar=scalar, in1=in1, op0=op0, op1=op1), reads, writes)

    def cp(self, eng, out, in_, reads, writes):
        self.P.add(eng, lambda e: e.tensor_copy(out, in_), reads, writes)

    def memset(self, eng, ap, val, reads, writes):
        self.P.add(eng, lambda e: e.memset(ap, val), reads, writes)

    def dma(self, eng, chan, out, in_, reads, writes):
        self.P.dma(eng, chan, lambda e: e.dma_start(out=out, in_=in_), reads, writes)


def build(NT, stage=3, debug=False):
    L = NT * 128
    nc = bass.Bass("TRN2", target_bir_lowering=False)

    def din(name, shape):
        return nc.dram_tensor(name, list(shape), F32, kind="ExternalInput").ap()

    x_d = din("x", [L, D])
    p_d = din("p", [L, DPLE])
    cs_d = din("cs", [L, 64])
    gA_d = din("gA", [128, 8])
    gF_d = din("gF", [128, 8])
    gP_d = din("gP", [128, 8])
    gfin_d = din("gfin", [128, D])
    gk_d = din("gk", [128, 64])
    par_d = din("cpar", [128, 4])
    sel2_d = din("csel2", [128, 64])
    w_in_d = din("w_in", [D, NIN])
    w_out_d = din("w_out", [D, D])
    w_gate_d = din("w_gate", [D, DFF])
    w_up_d = din("w_up", [D, DFF])
    w_down_d = din("w_down", [DFF, D])
    w_pg_d = din("w_pg", [D, D])
    w_pp_d = din("w_pp", [DPLE, D])
    out_d = nc.dram_tensor("out", [L, D], F32, kind="ExternalOutput").ap()
    skind = "ExternalOutput" if debug else "Internal"
    qs_d = nc.dram_tensor("qs", [L, QW], BF16, kind=skind).ap()
    h1_d = nc.dram_tensor("h1", [L, D], F32, kind=skind).ap()
    dbg = {}
    if debug:
        dbg["kaT"] = nc.dram_tensor("d_kaT", [128, L], BF16, kind="ExternalOutput").ap()
        dbg["kiT"] = nc.dram_tensor("d_kiT", [128, L], BF16, kind="ExternalOutput").ap()
        dbg["kbT"] = nc.dram_tensor("d_kbT", [128, 4, L], BF16, kind="ExternalOutput").ap()
        dbg["va1"] = nc.dram_tensor("d_va1", [128, NT, 65], BF16, kind="ExternalOutput").ap()
        dbg["vb1"] = nc.dram_tensor("d_vb1", [128, NT, 8, 65], BF16, kind="ExternalOutput").ap()
        dbg["kmT"] = nc.dram_tensor("d_kmT", [128, 4, 16], BF16, kind="ExternalOutput").ap()
        dbg["y"] = nc.dram_tensor("d_y", [L, D], BF16, kind="ExternalOutput").ap()
        dbg["kaT_end"] = nc.dram_tensor("d_kaT_end", [128, L], BF16, kind="ExternalOutput").ap()

    sb = SB(nc)
    P = Prog()
    k = KB(P)
    banks = [nc.alloc_psum_tensor("bank%d" % i, [128, 512], F32) for i in range(8)]
    bankb = [b[:, :].bitcast(BF16) for b in banks]

    def B(i):
        return ("b", i)

    ident = sb.alloc("ident", [128, 128], F32)
    identb = sb.alloc("identb", [128, 128], BF16)
    ident4 = sb.alloc("ident4", [128, 4, 128], BF16)
    tri = sb.alloc("tri", [128, 128], BF16)
    cpow = sb.alloc("cpow", [128, KIT + 2], F32)
    cm1 = sb.alloc("cm1", [128, 8, 1], F32)
    cmh = sb.alloc("cmh", [128, 1], F32)
    k.memset("pool", ident[:], 0.0, [], ["ident"])
    P.pool(lambda e: e.affine_select(out=ident[:], in_=ident[:], pattern=[[-1, 128]], compare_op=ALU.not_equal, fill=1.0,
                                     base=0, channel_multiplier=1), ["ident"], ["ident"])
    k.cp("dve", identb[:], ident[:], ["ident"], ["identb"])
    k.cp("dve", ident4[:], identb[:].unsqueeze(1).to_broadcast([128, 4, 128]), ["identb"], ["ident4"])
    k.memset("pool", tri[:], 0.0, [], ["tri"])
    P.pool(lambda e: e.affine_select(out=tri[:], in_=tri[:], pattern=[[1, 128]], compare_op=ALU.is_ge, fill=NEG,
                                     base=0, channel_multiplier=-1), ["tri"], ["tri"])
    for kk in range(KIT + 2):
        k.memset("pool", cpow[:, kk:kk + 1], float(2.0 ** (-(kk - 1))), [], ["cpow"])
    k.memset("pool", cm1[:], -1.0, [], ["cm1"])
    par = sb.alloc("par", [128, 4], F32)
    sel2f = sb.alloc("sel2f", [128, 64], F32)
    sel2 = sb.alloc("sel2", [128, 64], BF16)
    nsz = sb.alloc("nsz", [128, 4, 128], BF16)
    k.dma("sp", "par", par[:], par_d, [], ["par"])
    k.dma("sp", "sel2", sel2f[:], sel2_d, [], ["sel2f"])
    k.cp("dve", sel2[:], sel2f[:], ["sel2f"], ["sel2"])
    k.memset("pool", nsz[:], 0.0, [], ["nsz"])
    k.memset("pool", cmh[:], -0.5, [], ["cmh"])

    mark_glob = sb.mark()
    kaT2 = sb.alloc("kaT2", [128, L], BF16)
    kiT2 = sb.alloc("kiT2", [128, L], BF16)
    kbT = sb.alloc("kbT", [128, 4, L], BF16)
    va1 = sb.alloc("va1", [128, NT, 65], BF16)
    vb1 = sb.alloc("vb1", [128, NT, 8, 65], BF16)
    kmT = sb.alloc("kmT", [128, 4, 16], BF16)
    mark_phase = sb.mark()

    w_in = sb.alloc("w_in", [128, 8, NIN], BF16)
    gA = sb.alloc("gA", [128, 8], F32)
    gk = sb.alloc("gk", [128, 64], F32)
    xin = [sb.alloc("xin", [128, D], F32) for _ in range(2)]
    cst = [sb.alloc("cst", [128, 64], F32) for _ in range(2)]
    xT = sb.alloc("xT", [128, 8, 128], BF16)
    proj = [sb.alloc("proj", [128, NIN], F32) for _ in range(2)]
    rtmp = [sb.alloc("rtmp", [128, 30 * 32], F32) for _ in range(4)]
    qif = sb.alloc("qif", [128, 256], F32)
    kct = sb.alloc("kct", [128, 64], F32)
    knt = sb.alloc("knt", [128, 64], F32)
    ki1 = sb.alloc("ki1", [128, 64], BF16)
    ka2 = sb.alloc("ka2", [128, 128], BF16)
    ki2 = sb.alloc("ki2", [128, 128], BF16)
    qst = [sb.alloc("qst", [128, QW], BF16) for _ in range(2)]
    junkx = sb.alloc("junkx", [128, D], BF16)
    st = [sb.alloc("st", [128, 16], F32) for _ in range(2)]
    kmf = sb.alloc("kmf", [128, 4], F32)

    for hf in range(2):
        k.dma("pool", "win%d" % hf, w_in[:, hf * 4:(hf + 1) * 4, :],
              w_in_d[hf * 512:(hf + 1) * 512, :].rearrange("(c p) n -> p c n", p=128), [], [("w_in", hf)])
    k.dma("sp", "gA", gA[:], gA_d, [], ["gA"])
    k.dma("sp", "gk", gk[:], gk_d, [], ["gk"])
    k.memset("pool", va1[:], 1.0, [], ["va1init"])
    k.memset("pool", vb1[:], 1.0, [], ["vb1init"])
    k.memset("pool", kmT[:], 0.0, [], ["kmT"])

    def rope(src, dst, H, t0, cs, rsrc, wdst, tag):
        def v(ap):
            return ap.rearrange("p (h t d) -> p h t d", t=2, d=32)

        x1 = v(src)[:, :, 0, :]
        x2 = v(src)[:, :, 1, :]
        d1 = v(dst)[:, :, 0, :]
        d2 = v(dst)[:, :, 1, :]
        cos = cs[:, 0:32].unsqueeze(1).to_broadcast([128, H, 32])
        sin = cs[:, 32:64].unsqueeze(1).to_broadcast([128, H, 32])
        tv = [t[:, t0 * 32:(t0 + H) * 32].rearrange("p (h d) -> p h d", d=32) for t in rtmp]
        rn = [("rt", n, tag) for n in range(4)]
        k.tt("dve", tv[0], x1, cos, ALU.mult, rsrc, [rn[0]])
        k.tt("pool", tv[1], x2, sin, ALU.mult, rsrc, [rn[1]])
        k.tt("dve", d1, tv[0], tv[1], ALU.subtract, [rn[0], rn[1]], wdst)
        k.tt("pool", tv[2], x2, cos, ALU.mult, rsrc, [rn[2]])
        k.tt("dve", tv[3], x1, sin, ALU.mult, rsrc, [rn[3]])
        k.tt("pool", d2, tv[2], tv[3], ALU.add, [rn[2], rn[3]], wdst)

    def a0_front(i):
        b = i % 2
        r0, r1 = i * 128, (i + 1) * 128
        P.tag = "A0.stats"
        k.dma("sp", "x%d" % b, xin[b][:], x_d[r0:r1, :], [], [("xin", b)])
        k.dma("sp", "cs%d" % b, cst[b][:], cs_d[r0:r1, :], [], [("cst", b)])
        S = st[b]
        sr = lambda c: ("st", b, c)
        k.actf(junkx[:], xin[b][:], AF.Square, [("xin", b)], ["junkx", sr(0)], accum=S[:, 0:1])
        k.ts("dve", S[:, 1:2], S[:, 0:1], 1.0 / D, EPS, ALU.mult, ALU.add, [sr(0)], [sr(1)])
        k.tt("pool", S[:, 2:3], S[:, 1:2], cmh[:], ALU.pow, [sr(1), "cmh"], [sr(2)])
        P.tag = "A0.xT"
        for kc in range(8):
            k.tr(banks[kc // 4][:, (kc % 4) * 128:(kc % 4 + 1) * 128], xin[b][:, kc * 128:(kc + 1) * 128], ident[:],
                 [("xin", b), "ident"], [B(kc // 4)])
        for hf in range(2):
            k.tt("dve", xT[:, hf * 4:(hf + 1) * 4, :], banks[hf][:, :].rearrange("p (c t) -> p c t", c=4),
                 gA[:, hf * 4:(hf + 1) * 4].unsqueeze(2).to_broadcast([128, 4, 128]), ALU.mult, [B(hf), "gA"], [("xT", hf)])
        P.tag = "A0.inproj"
        pj = proj[b]
        for nb in range(5):
            n0 = nb * 512
            n1 = min(NIN, n0 + 512)
            w = n1 - n0
            for kc in range(8):
                k.mm(banks[2 + nb][:, 0:w], xT[:, kc, :], w_in[:, kc, n0:n1], kc == 0, kc == 7,
                     [("xT", kc // 4), ("w_in", kc // 4)], [B(2 + nb)])
            k.actf(pj[:, n0:n1], banks[2 + nb][:, 0:w], AF.Copy, [B(2 + nb), sr(2)], [("proj", b, nb)], scale=S[:, 2:3])
    def a0_back(i):
        b = i % 2
        r0, r1 = i * 128, (i + 1) * 128
        S = st[b]
        sr = lambda c: ("st", b, c)
        pj = proj[b]
        P.tag = "A0.rope"
        pall = [("proj", b, nb) for nb in range(5)]
        Q = qst[b]
        qr = ("qst", b)
        rope(pj[:, 0:576], Q[:, 0:576], 9, 0, cst[b], pall[0:2] + [("cst", b)], [qr], "a")
        k.cp("pool", ka2[:].rearrange("p (c d) -> p c d", c=2), Q[:, S_KA:S_KA + 64].unsqueeze(1).to_broadcast([128, 2, 64]),
             [qr], ["ka2"])
        rope(pj[:, C_QB:C_QB + 1024], Q[:, S_QB:S_QB + 1024], 16, 9, cst[b], pall[1:4] + [("cst", b)], [qr], "b")
        k.actf(S[:, 8:12], pj[:, C_WI:C_WI + 4], AF.Abs, [pall[1]], [sr(8)], scale=0.125)
        k.ts("dve", Q[:, S_SG:S_SG + 4], pj[:, C_WI:C_WI + 4], 0.0, -0.5, ALU.is_ge, ALU.add, [pall[1]], [qr])
        rope(pj[:, C_QI:C_QI + 256], qif[:], 4, 25, cst[b], [pall[1], ("cst", b)], ["qif"], "i")
        k.tt("dve", Q[:, S_QI:S_QI + 256].rearrange("p (h d) -> p h d", h=4), qif[:].rearrange("p (h d) -> p h d", h=4),
             S[:, 8:12].unsqueeze(2).to_broadcast([128, 4, 64]), ALU.mult, ["qif", sr(8)], [qr])
        P.dve(lambda e, S=S, pj=pj: e.tensor_reduce(out=S[:, 3:4], in_=pj[:, C_KI:C_KI + 64], axis=AX.X, op=ALU.add),
              [pall[1]], [sr(3)])
        k.ts("dve", S[:, 4:5], S[:, 3:4], -1.0 / 64, None, ALU.mult, None, [sr(3)], [sr(4)])
        k.ts("dve", kct[:], pj[:, C_KI:C_KI + 64], S[:, 4:5], None, ALU.add, None, [pall[1], sr(4)], ["kct"])
        k.actf(junkx[:, 0:64], kct[:], AF.Square, ["kct"], ["junkx", sr(5)], accum=S[:, 5:6])
        k.ts("dve", S[:, 6:7], S[:, 5:6], 1.0 / 64, EPS, ALU.mult, ALU.add, [sr(5)], [sr(6)])
        k.tt("pool", S[:, 7:8], S[:, 6:7], cmh[:], ALU.pow, [sr(6), "cmh"], [sr(7)])
        k.stt(knt[:], kct[:], S[:, 7:8], gk[:], ALU.mult, ALU.mult, ["kct", sr(7), "gk"], ["knt"])
        rope(knt[:], ki1[:], 1, 29, cst[b], ["knt", ("cst", b)], ["ki1"], "k")
        k.cp("pool", ki2[:].rearrange("p (c d) -> p c d", c=2), ki1[:].unsqueeze(1).to_broadcast([128, 2, 64]), ["ki1"], ["ki2"])
        P.tag = "A0.kT"
        k.actf(va1[:, i, 0:64], pj[:, C_VA:C_VA + 64], AF.Copy, [pall[1], "va1init"], [("va1", i)])
        k.actf(vb1[:, i, :, 0:64], pj[:, C_VB:C_VB + 512].rearrange("p (h d) -> p h d", h=8), AF.Copy,
               [pall[3], pall[4], "vb1init"], [("vb1", i)])
        tp = bankb[7]
        k.tr(tp[:, 0:128], ka2[:], identb[:], ["ka2", "identb"], [B(7)])
        k.tr(tp[:, 128:256], ki2[:], identb[:], ["ki2", "identb"], [B(7)])
        for pp in range(4):
            k.tr(tp[:, 256 + pp * 128:384 + pp * 128], Q[:, S_KB + pp * 128:S_KB + (pp + 1) * 128], identb[:], [qr, "identb"], [B(7)])
        k.actf(kaT2[:, r0:r1], tp[:, 0:128], AF.Copy, [B(7)], [("kaT", i)])
        k.cp("dve", kiT2[:, r0:r1], tp[:, 128:256], [B(7)], [("kiT", i)])
        k.actf(kbT[:, :, r0:r1], tp[:, 256:768].rearrange("p (c t) -> p c t", c=4), AF.Copy, [B(7)], [("kbT", i)])
        if i % 2 == 1:
            j = i // 2
            P.dve(lambda e, j=j: e.tensor_reduce(out=kmf[:], in_=kbT[:, :, j * 256:(j + 1) * 256], axis=AX.X, op=ALU.add),
                  [("kbT", i - 1), ("kbT", i)], ["kmf"])
            k.ts("dve", kmT[:, :, j], kmf[:], 1.0 / 256, None, ALU.mult, None, ["kmf", "kmT"], ["kmT"])
        k.dma("sp", "qs%d" % b, qs_d[r0:r1, :], Q[:], [qr], [("qs_d", i)])

    a0_front(0)
    for i in range(NT):
        if i + 1 < NT:
            a0_front(i + 1)
        a0_back(i)

    if debug:
        P.barrier()
        k.dma("sp", "dbg", dbg["kaT"], kaT2[:], [], [])
        k.dma("sp", "dbg", dbg["kiT"], kiT2[:], [], [])
        k.dma("sp", "dbg", dbg["kbT"], kbT[:], [], [])
        k.dma("sp", "dbg", dbg["va1"], va1[:], [], [])
        k.dma("sp", "dbg", dbg["vb1"], vb1[:], [], [])
        k.dma("sp", "dbg", dbg["kmT"], kmT[:], [], [])
    if stage < 2:
        P.emit(nc)
        return nc, sb

    P.barrier()
    sb.reset(mark_phase)
    w_out = sb.alloc("w_out", [128, 8, D], BF16)
    qin = [sb.alloc("qin", [128, QW], BF16) for _ in range(2)]
    xin = [sb.alloc("xin1", [128, D], F32) for _ in range(2)]
    qaT = [sb.alloc("qaT", [64, 1024], BF16) for _ in range(2)]
    qbT = [sb.alloc("qbT", [128, 4, 128], BF16) for _ in range(2)]
    qiT = sb.alloc("qiT", [64, 512], BF16)
    dgs = sb.alloc("dgs", [128, 4, 128], BF16)
    rl = [sb.alloc("rl", [128, 4, 256], BF16) for _ in range(2)]
    sc = sb.alloc("sc", [128, L], F32)
    junk = sb.alloc("junk", [128, L], BF16)
    mb = [sb.alloc("mb", [128, L], BF16) for _ in range(2)]
    sst = [sb.alloc("sst", [128, 40], F32) for _ in range(2)]
    steps2 = sb.alloc("steps2", [128, KIT + 2], F32)
    mids = sb.alloc("mids", [128, KIT + 2], F32)
    cnts = sb.alloc("cnts", [128, KIT + 2], F32)
    sks = sb.alloc("sks", [128, KIT + 2], F32)
    gate = sb.alloc("gate", [128, 8, 16], F32)
    top8 = sb.alloc("top8", [128, 8, 8], F32)
    nsf = sb.alloc("nsf", [128, 8, 16], F32)
    negsel = sb.alloc("negsel", [128, 128], BF16)
    NS4 = [sb.alloc("NS4", [128, 4, 128], BF16) for _ in range(2)]
    PTe = [sb.alloc("PTe", [128, 512], BF16) for _ in range(2)]
    PTo = [sb.alloc("PTo", [128, 512], BF16) for _ in range(2)]
    ysb = [sb.alloc("ysb", [128, 8, 65], F32) for _ in range(2)]
    rec = [sb.alloc("rec", [128, 8, 1], F32) for _ in range(2)]
    yt = sb.alloc("y", [128, D], BF16)
    yT = sb.alloc("yT", [128, 8, 128], BF16)
    tmp = sb.alloc("tmp", [128, D], F32)
    h1t = [sb.alloc("h1t", [128, D], F32) for _ in range(2)]

    k.dma("pool", "wout", w_out[:], w_out_d.rearrange("(c p) n -> p c n", p=128), [], ["w_out"])

    def a1_loads(i):
        b = i % 2
        k.dma("sp", "q%d" % b, qin[b][:], qs_d[i * 128:(i + 1) * 128, :], [], [("qin", b)])
        k.dma("sp", "x1%d" % b, xin[b][:], x_d[i * 128:(i + 1) * 128, :], [], [("xin", b)])

    def a1_front(i):
        b = i % 2
        j = i // 2
        Lk = (i + 1) * 128
        Qn = qin[b]
        qr = ("qin", b)
        P.tag = "A1.T"
        for h in range(8):
            k.tr(bankb[6][0:64, h * 128:(h + 1) * 128], Qn[:, S_QA + h * 64:S_QA + (h + 1) * 64], identb[:], [qr], [B(6)])
        for pp in range(4):
            k.tr(bankb[7][:, pp * 128:(pp + 1) * 128], Qn[:, S_QB + pp * 128:S_QB + (pp + 1) * 128], identb[:], [qr], [B(7)])
        for h in range(4):
            k.tr(bankb[7][0:64, 512 + h * 128:512 + (h + 1) * 128], Qn[:, S_QI + h * 64:S_QI + (h + 1) * 64], identb[:], [qr], [B(7)])
        k.actf(qaT[b][:], bankb[6][0:64, :], AF.Copy, [B(6)], [("qaT", b)])
        k.actf(qbT[b][:], bankb[7][:, 0:512].rearrange("p (c t) -> p c t", c=4), AF.Copy, [B(7)], [("qbT", b)])
        k.actf(qiT[:], bankb[7][0:64, 512:1024], AF.Copy, [B(7)], ["qiT"])
        for h in range(4):
            k.ts("dve", dgs[:, h, :], identb[:], Qn[:, S_SG + h:S_SG + h + 1], None, ALU.mult, None, [qr], [("dgs", h)])
        if j >= 1:
            P.tag = "A1.gate"
            for g in range(2):
                P.act(lambda e, g=g: e.memzero(banks[6 + g][:, 0:64]), [], [B(6 + g)])
            for g in range(2):
                for pp in range(4):
                    k.mm(banks[6 + g][:, pp * 16:(pp + 1) * 16], qbT[b][g * 64:(g + 1) * 64, pp, :], kmT[g * 64:(g + 1) * 64, pp, :],
                         False, False, [("qbT", b)], [B(6 + g)], skip=True)
            for g in range(2):
                k.actf(gate[:, g * 4:(g + 1) * 4, :], banks[6 + g][:, 0:64].rearrange("p (c n) -> p c n", c=4), AF.Copy,
                       [B(6 + g)], ["gate"])
            if j < 16:
                k.memset("pool", gate[:, :, j:16], -BIG, ["gate"], ["gate"])
            for hh in range(8):
                P.dve(lambda e, hh=hh: e.max(out=top8[:, hh, :], in_=gate[:, hh, :]), ["gate"], [("top8", hh)])
            k.tt("dve", nsf[:], gate[:], top8[:, :, 2:3].to_broadcast([128, 8, 16]), ALU.is_lt,
                 ["gate"] + [("top8", hh) for hh in range(8)], ["nsf"])
            if j < 16:
                k.memset("dve", nsf[:, :, j:16], 0.0, ["nsf"], ["nsf"])
            k.ts("dve", negsel[:], nsf[:].rearrange("p h n -> p (h n)"), NEG, None, ALU.mult, None, ["nsf"], ["negsel"])
        if WARMUP > 0:
            P.tag = "A1.warm"
            for wq in range(WARMUP):
                k.mm(banks[4 + wq % 2][:, :], ident4[:, wq % 4, :], ident4[:], True, True, [], [B(4 + wq % 2)])
        P.tag = "A1.idx"
        SS = sst[b]
        sr = ("sst", b)
        nch = (Lk + 255) // 256

        def Zc(c):
            c0 = c * 256
            w = min(256, Lk - c0)
            st_ = c % 2
            for h in range(4):
                bk = st_ * 2 + h // 2
                col = (h % 2) * 256
                k.mm(banks[bk][:, col:col + w], qiT[:, h * 128:(h + 1) * 128], kiT2[0:64, c0:c0 + w], True, True,
                     ["qiT"], [B(bk)], skip=True)
            for hb in range(2):
                bk = st_ * 2 + hb
                k.actf(rl[st_][:, hb * 2:(hb + 1) * 2, 0:w], banks[bk][:, :].rearrange("p (h x) -> p h x", h=2)[:, :, 0:w], AF.Relu,
                       [B(bk)], [("rl", st_, hb)])

        def SSc(c):
            c0 = c * 256
            w = min(256, Lk - c0)
            st_ = c % 2
            sbk = 6 + c % 2
            for h in range(4):
                k.mm(banks[sbk][:, 0:w], dgs[:, h, :], rl[st_][:, h, 0:w], h == 0, h == 3, [("dgs", h), ("rl", st_, h // 2)], [B(sbk)])
            k.ts("dve", sc[:, c0:c0 + w], banks[sbk][:, 0:w], 1.0, -3.0e38, ALU.mult, ALU.max, [B(sbk)], ["sc", sr],
                 accum=SS[:, c:c + 1])
            k.ts("dve", junk[:, c0:c0 + w], sc[:, c0:c0 + w], 1.0, 3.0e38, ALU.mult, ALU.min, ["sc"], ["junk", sr],
                 accum=SS[:, 16 + c:17 + c])

        Zc(0)
        for c in range(1, nch):
            Zc(c)
            SSc(c - 1)
        SSc(nch - 1)
        if j >= 1:
            P.tag = "A1.gate"
            k.tr(bankb[7][:, 0:128], negsel[:], identb[:], ["negsel"], [B(7)])
            k.tt("dve", NS4[b][:], bankb[7][:, 0:128].unsqueeze(1).to_broadcast([128, 4, 128]),
                 par[:].unsqueeze(2).to_broadcast([128, 4, 128]), ALU.mult, [B(7), "par"], [("NS4", b)])
        P.tag = "A1.search"
        if i >= 2:
            if nch > 1:
                P.dve(lambda e: e.tensor_reduce(out=SS[:, 32:33], in_=SS[:, 0:nch], axis=AX.X, op=ALU.max), [sr], [sr])
                P.dve(lambda e: e.tensor_reduce(out=SS[:, 33:34], in_=SS[:, 16:16 + nch], axis=AX.X, op=ALU.min), [sr], [sr])
                cmax, cmin = SS[:, 32:33], SS[:, 33:34]
            else:
                cmax, cmin = SS[:, 0:1], SS[:, 16:17]
            k.tt("dve", SS[:, 34:35], cmax, cmin, ALU.subtract, [sr], [sr])
            k.ts("dve", steps2[:], cpow[:], SS[:, 34:35], None, ALU.mult, None, [sr, "cpow"], ["steps2"])
            k.stt(mids[:, 1:2], SS[:, 34:35], 0.5, cmin, ALU.mult, ALU.add, [sr], ["mids"])
        P.pool(lambda e: e.affine_select(out=sc[:, i * 128:(i + 1) * 128], in_=sc[:, i * 128:(i + 1) * 128], pattern=[[-1, 128]],
                                         compare_op=ALU.is_ge, fill=-BIG, base=0, channel_multiplier=1), ["sc"], ["sc"])
        if i >= 2:
            for kk in range(1, KIT + 1):
                k.ts("dve", junk[:, 0:Lk], sc[:, 0:Lk], mids[:, kk:kk + 1], 0.0, ALU.is_ge, ALU.add, ["sc", "mids"], ["junk", "cnts"],
                     accum=cnts[:, kk:kk + 1])
                k.ts("dve", sks[:, kk:kk + 1], cnts[:, kk:kk + 1], 255.5, -0.5, ALU.is_ge, ALU.add, ["cnts"], ["sks"])
                k.stt(mids[:, kk + 1:kk + 2], sks[:, kk:kk + 1], steps2[:, kk + 1:kk + 2], mids[:, kk:kk + 1], ALU.mult, ALU.add,
                      ["sks", "steps2", "mids"], ["mids"])
            k.stt(SS[:, 35:36], steps2[:, KIT + 1:KIT + 2], -0.5, mids[:, KIT + 1:KIT + 2], ALU.mult, ALU.add, ["steps2", "mids"], [sr])
        else:
            k.memset("dve", SS[:, 35:36], -1.0e29, [sr], [sr])
        P.tag = "A1.mask"
        k.ts("dve", mb[b][:, 0:Lk], sc[:, 0:Lk], SS[:, 35:36], NEG, ALU.is_lt, ALU.mult, ["sc", sr], [("mb", b)])

    cc_state = [0]

    def a1_back(i):
        b = i % 2
        j = i // 2
        nkt = i + 1
        YM = [banks[4], banks[5]]
        YD = [banks[6], banks[7]]
        ystart_m = [True, True]
        ystart_d = [True, True]
        steps = []
        for pp in range(4):
            for c in range((nkt + 3) // 4):
                steps.append(("m", pp, c))
        n_moba = len(steps)
        for kt in range(nkt):
            steps.append(("d", kt, 0))

        def S(step, s):
            kind, a0, a1 = step
            if kind == "m":
                pp, c = a0, a1
                E, O = banks[s], banks[2 + s]
                kts = list(range(4 * c, min(4 * c + 4, nkt)))
                w = len(kts) * 128
                rhs_ns = (NS4[b] if j >= 1 else nsz)[:, 0:len(kts), :]
                rd_ns = [("NS4", b)] if j >= 1 else []
                for (bank, bi, hh, g) in ((E, s, pp, 0), (O, 2 + s, 4 + pp, 1)):
                    P.tag = "A1.moba.bias"
                    k.mm(bank[:, 0:w], sel2[:, hh * 8 + c:hh * 8 + c + 1].to_broadcast([128, 128]), rhs_ns, True, False,
                         rd_ns, [B(bi)], skip=True)
                    P.tag = "A1.moba.s"
                    for kt in kts:
                        col = (kt % 4) * 128
                        k.mm(bank[:, col:col + 128], kbT[g * 64:(g + 1) * 64, pp, kt * 128:(kt + 1) * 128],
                             qbT[b][g * 64:(g + 1) * 64, pp, :], False, False, [("qbT", b)], [B(bi)], skip=True)
                        if kt == i:
                            k.mm(bank[:, col:col + 128], identb[:], tri[:], False, False, [], [B(bi)], skip=True)
                P.tag = "A1.moba.exp"
                k.actf(PTe[s][:, 0:w], E[:, 0:w], AF.Exp, [B(s)], [("PTe", s)], scale=0.125)
                k.actf(PTo[s][:, 0:w], O[:, 0:w], AF.Exp, [B(2 + s)], [("PTo", s)], scale=0.125)
            else:
                kt = a0
                SA, SBk = banks[s], banks[2 + s]
                ks = slice(kt * 128, (kt + 1) * 128)
                P.tag = "A1.dsa.s"
                k.mm(SA[:, :], kaT2[0:64, ks], qaT[b][:, 0:512], True, False, [("qaT", b)], [B(s)])
                P.tag = "A1.dsa.mask"
                k.mm(SA[:, :], mb[b][:, ks], ident4[:], False, True, [("mb", b)], [B(s)])
                P.tag = "A1.dsa.s"
                k.mm(SBk[:, :], kaT2[0:64, ks], qaT[b][:, 512:1024], True, False, [("qaT", b)], [B(2 + s)])
                P.tag = "A1.dsa.mask"
                k.mm(SBk[:, :], mb[b][:, ks], ident4[:], False, True, [("mb", b)], [B(2 + s)])
                P.tag = "A1.dsa.exp"
                k.actf(PTe[s][:], SA[:, :], AF.Exp, [B(s)], [("PTe", s)], scale=0.125)
                k.actf(PTo[s][:], SBk[:, :], AF.Exp, [B(2 + s)], [("PTo", s)], scale=0.125)

        def V(step, s):
            kind, a0, a1 = step
            if kind == "m":
                pp, c = a0, a1
                he, ho = 2 * pp, 2 * pp + 1
                kts = list(range(4 * c, min(4 * c + 4, nkt)))
                P.tag = "A1.moba.pv"
                for kt in kts:
                    col = (kt % 4) * 128
                    for (PT, pn, h) in ((PTe[s], ("PTe", s), he), (PTo[s], ("PTo", s), ho)):
                        yb_ = h // 4
                        k.mm(YM[yb_][:, (h % 4) * 65:(h % 4) * 65 + 65], PT[:, col:col + 128], vb1[:, kt, h, :], False, False,
                             [pn], [B(4 + yb_)], skip=True)
                        ystart_m[yb_] = False
            else:
                kt = a0
                P.tag = "A1.dsa.pv"
                for pp in range(4):
                    for (PT, pn, h) in ((PTe[s], ("PTe", s), pp), (PTo[s], ("PTo", s), 4 + pp)):
                        yb_ = h // 4
                        k.mm(YD[yb_][:, (h % 4) * 65:(h % 4) * 65 + 65], PT[:, pp * 128:(pp + 1) * 128], va1[:, kt, :], False, False,
                             [pn], [B(6 + yb_)], skip=True)
                        ystart_d[yb_] = False

        def norm(which):
            Yb, y0, b0, ys, tagn = (YM, 512, 4, 0, "A1.moba.norm") if which == "m" else (YD, 0, 6, 1, "A1.dsa.norm")
            P.tag = tagn
            for yb_ in range(2):
                k.actf(ysb[ys][:, yb_ * 4:(yb_ + 1) * 4, :], Yb[yb_][:, 0:260].rearrange("p (h d) -> p h d", h=4), AF.Copy,
                       [B(b0 + yb_)], [("ysb", ys)])
            k.tt("pool", rec[ys][:], ysb[ys][:, :, 64:65], cm1[:], ALU.pow, [("ysb", ys)], [("rec", ys)])
            k.tt("pool", yt[:, y0:y0 + 512].rearrange("p (h d) -> p h d", h=8), ysb[ys][:, :, 0:64], rec[ys][:].to_broadcast([128, 8, 64]),
                 ALU.mult, [("ysb", ys), ("rec", ys)], [("y", 1 - ys)])

        P.tag = "A1.yzero"
        for yb_ in range(4):
            P.act(lambda e, yb_=yb_: e.memzero(banks[4 + yb_][:, 0:260]), [], [B(4 + yb_)])
        sets = []
        for n, st_ in enumerate(steps):
            s_ = cc_state[0] % 2
            cc_state[0] += 1
            sets.append(s_)
            S(st_, s_)
            if n >= 1:
                V(steps[n - 1], sets[n - 1])
                if n == n_moba:
                    norm("m")
        V(steps[-1], sets[-1])
        norm("d")
        if debug:
            k.dma("sp", "dbgy", dbg["y"][i * 128:(i + 1) * 128, :], yt[:], [("y", 0), ("y", 1)], [])
        P.tag = "A1.oproj"
        for kc in range(8):
            k.tr(bankb[5][:, kc * 128:(kc + 1) * 128], yt[:, kc * 128:(kc + 1) * 128], identb[:], [("y", kc // 4)], [B(5)])
        k.actf(yT[:], bankb[5][:, :].rearrange("p (c t) -> p c t", c=8), AF.Copy, [B(5)], ["yT"])
        for nb in range(2):
            for kc in range(8):
                k.mm(banks[4 + nb][:, :], yT[:, kc, :], w_out[:, kc, nb * 512:(nb + 1) * 512], kc == 0, kc == 7,
                     ["yT", "w_out"], [B(4 + nb)])
            k.actf(tmp[:, nb * 512:(nb + 1) * 512], banks[4 + nb][:, :], AF.Copy, [B(4 + nb)], [("tmp", nb)])
        k.tt("pool", h1t[b][:], tmp[:], xin[b][:], ALU.add, [("tmp", 0), ("tmp", 1), ("xin", b)], [("h1t", b)])
        k.dma("sp", "h1%d" % b, h1_d[i * 128:(i + 1) * 128, :], h1t[b][:], [("h1t", b)], [("h1_d", i)])

    def load_q(i):
        b = i % 2
        k.dma("sp", "q%d" % b, qin[b][:], qs_d[i * 128:(i + 1) * 128, :], [], [("qin", b)])

    def load_x(i):
        b = i % 2
        k.dma("sp", "x1%d" % b, xin[b][:], x_d[i * 128:(i + 1) * 128, :], [], [("xin", b)])

    load_q(0)
    for i in range(NT + 1):
        if i < NT:
            a1_front(i)
        if i + 1 < NT:
            load_q(i + 1)
        if i >= 1:
            load_x(i - 1)
            a1_back(i - 1)
    if debug:
        P.barrier()
        k.dma("sp", "dbg2", dbg["kaT_end"], kaT2[:], [], [])
    if stage < 3:
        P.emit(nc)
        nc._prog_names = P.names
        return nc, sb

    P.barrier()
    sb.reset(mark_glob)
    wg = sb.alloc("wg", [128, 8, DFF], BF16)
    wu = sb.alloc("wu", [128, 8, DFF], BF16)
    wd = sb.alloc("wd", [128, NFC, D], BF16)
    wpg = sb.alloc("wpg", [128, 8, D], BF16)
    wpp = sb.alloc("wpp", [128, 2, D], BF16)
    gF = sb.alloc("gF", [128, 8], F32)
    gP = sb.alloc("gP", [128, 8], F32)
    gfin = sb.alloc("gfin", [128, D], F32)
    hb = [sb.alloc("hb", [128, 2, D], F32) for _ in range(2)]
    pb = [sb.alloc("pb", [128, 2, DPLE], F32) for _ in range(2)]
    hn = sb.alloc("hn", [128, D], BF16)
    fT = sb.alloc("fT", [128, 8, 256], BF16)
    actT = sb.alloc("actT", [128, NFC, 256], BF16)
    sgt = [sb.alloc("sgt", [128, 256], F32) for _ in range(2)]
    gtt = [sb.alloc("gtt", [128, 256], F32) for _ in range(2)]
    pn = sb.alloc("pn", [128, 2, DPLE], BF16)
    pT = sb.alloc("pT", [128, 2, 256], BF16)
    sgp = sb.alloc("sgp", [128, 512], F32)
    tpp = sb.alloc("tpp", [128, 512], F32)
    stB = [sb.alloc("stB", [128, 32], F32) for _ in range(2)]

    k.dma("sp", "gF", gF[:], gF_d, [], ["gF"])
    k.dma("sp", "gP", gP[:], gP_d, [], ["gP"])
    k.dma("sp", "gfin", gfin[:], gfin_d, [], ["gfin"])
    HB = DFF // 2
    for blk in range(2):
        cs_ = slice(blk * HB, (blk + 1) * HB)
        k.dma("pool", "wg%d" % blk, wg[:, :, cs_], w_gate_d[:, cs_].rearrange("(c p) n -> p c n", p=128), [], [("wg", blk)])
        k.dma("pool", "wu%d" % blk, wu[:, :, cs_], w_up_d[:, cs_].rearrange("(c p) n -> p c n", p=128), [], [("wu", blk)])
    for blk in range(2):
        k.dma("pool", "wd%d" % blk, wd[:, blk * 11:(blk + 1) * 11, :],
              w_down_d[blk * 1408:(blk + 1) * 1408, :].rearrange("(c p) n -> p c n", p=128), [], [("wd", blk)])
    k.dma("pool", "wpg", wpg[:], w_pg_d.rearrange("(c p) n -> p c n", p=128), [], ["wpg"])
    k.dma("pool", "wpp", wpp[:], w_pp_d.rearrange("(c p) n -> p c n", p=128), [], ["wpp"])

    NTB = NT // 2

    def b_loads(i):
        b = i % 2
        r0 = i * 256
        k.dma("sp", "hb%d" % b, hb[b][:], h1_d[r0:r0 + 256, :].rearrange("(s p) d -> p s d", p=128), [("h1_d", 2 * i), ("h1_d", 2 * i + 1)],
              [("hb", b, 0), ("hb", b, 1)])
        k.dma("sp", "pb%d" % b, pb[b][:], p_d[r0:r0 + 256, :].rearrange("(s p) d -> p s d", p=128), [], [("pb", b)])

    def norm_T(H, b, sub, S, c0, gvec, gname, dstT):
        hr = ("hb", b, sub)
        sr = lambda c: ("stB", b, c0 + c)
        k.actf(hn[:], H[:, sub, :], AF.Square, [hr], ["hn", sr(0)], accum=S[:, c0:c0 + 1])
        k.ts("dve", S[:, c0 + 1:c0 + 2], S[:, c0:c0 + 1], 1.0 / D, EPS, ALU.mult, ALU.add, [sr(0)], [sr(1)])
        k.tt("pool", S[:, c0 + 2:c0 + 3], S[:, c0 + 1:c0 + 2], cmh[:], ALU.pow, [sr(1)], [sr(2)])
        k.actf(hn[:], H[:, sub, :], AF.Copy, [hr, sr(2)], ["hn"], scale=S[:, c0 + 2:c0 + 3])
        for kc in range(8):
            k.tr(bankb[0][:, kc * 128:(kc + 1) * 128], hn[:, kc * 128:(kc + 1) * 128], identb[:], ["hn"], [B(0)])
        k.tt("dve", dstT[:, :, sub * 128:(sub + 1) * 128], bankb[0][:, :].rearrange("p (c t) -> p c t", c=8),
             gvec[:].unsqueeze(2).to_broadcast([128, 8, 128]), ALU.mult, [B(0), gname], [("fT", sub)])

    b_loads(0)
    for i in range(NTB):
        b = i % 2
        H = hb[b]
        S = stB[b]
        if i + 1 < NTB:
            b_loads(i + 1)
        P.tag = "B.norm"
        for sub in range(2):
            norm_T(H, b, sub, S, sub * 4, gF, "gF", fT)
        P.tag = "B.gu"
        for fc in range(NFC):
            s2 = fc % 2
            G, U = banks[2 + 2 * s2], banks[3 + 2 * s2]
            fs = slice(fc * 128, (fc + 1) * 128)
            for kc in range(8):
                k.mm(G[:, 0:256], wg[:, kc, fs], fT[:, kc, :], kc == 0, kc == 7, [("wg", fc // 11), ("fT", 0), ("fT", 1)], [B(2 + 2 * s2)])
            for kc in range(8):
                k.mm(U[:, 0:256], wu[:, kc, fs], fT[:, kc, :], kc == 0, kc == 7, [("wu", fc // 11), ("fT", 0), ("fT", 1)], [B(3 + 2 * s2)])
            k.actf(sgt[s2][:], G[:, 0:256], AF.Sigmoid, [B(2 + 2 * s2)], [("sgt", s2)])
            k.tt("dve", gtt[s2][:], G[:, 0:256], sgt[s2][:], ALU.mult, [B(2 + 2 * s2), ("sgt", s2)], [("gtt", s2)])
            k.tt("dve", actT[:, fc, :], gtt[s2][:], U[:, 0:256], ALU.mult, [B(3 + 2 * s2), ("gtt", s2)], [("actT", fc)])
        P.tag = "B.down"
        for sub in range(2):
            for nb in range(2):
                bk = 6 + (sub * 2 + nb) % 2
                for fc in range(NFC):
                    k.mm(banks[bk][:, :], actT[:, fc, sub * 128:(sub + 1) * 128], wd[:, fc, nb * 512:(nb + 1) * 512], fc == 0, fc == NFC - 1,
                         [("actT", fc), ("wd", fc // 11)], [B(bk)])
                hs = H[:, sub, nb * 512:(nb + 1) * 512]
                k.tt("dve", hs, banks[bk][:, :], hs, ALU.add, [B(bk), ("hb", b, sub)], [("hb", b, sub)])
        P.tag = "B.ple"
        k.cp("pool", pn[:], pb[b][:], [("pb", b)], ["pn"])
        for sub in range(2):
            for c in range(2):
                k.tr(bankb[1][:, (sub * 2 + c) * 128:(sub * 2 + c + 1) * 128], pn[:, sub, c * 128:(c + 1) * 128], identb[:], ["pn"], [B(1)])
        k.actf(pT[:].rearrange("p c (s t) -> p s c t", s=2), bankb[1][:, 0:512].rearrange("p (s c t) -> p s c t", s=2, c=2), AF.Copy,
               [B(1)], ["pT"])
        for sub in range(2):
            norm_T(H, b, sub, S, 8 + sub * 4, gP, "gP", fT)
        for sub in range(2):
            for nb in range(2):
                s2 = (sub * 2 + nb) % 2
                PG, PP = banks[2 + 2 * s2], banks[3 + 2 * s2]
                ns = slice(nb * 512, (nb + 1) * 512)
                for kc in range(8):
                    k.mm(PG[:, :], fT[:, kc, sub * 128:(sub + 1) * 128], wpg[:, kc, ns], kc == 0, kc == 7, [("fT", sub), "wpg"],
                         [B(2 + 2 * s2)])
                for c in range(2):
                    k.mm(PP[:, :], pT[:, c, sub * 128:(sub + 1) * 128], wpp[:, c, ns], c == 0, c == 1, ["pT", "wpp"], [B(3 + 2 * s2)])
                k.actf(sgp[:], PG[:, :], AF.Sigmoid, [B(2 + 2 * s2)], ["sgp"])
                k.tt("dve", tpp[:], sgp[:], PP[:, :], ALU.mult, ["sgp", B(3 + 2 * s2)], ["tpp"])
                hs = H[:, sub, ns]
                k.tt("pool", hs, hs, tpp[:], ALU.add, ["tpp", ("hb", b, sub)], [("hb", b, sub)])
        P.tag = "B.fin"
        for sub in range(2):
            c0 = 16 + sub * 4
            hr = ("hb", b, sub)
            sr = lambda c: ("stB", b, c0 + c)
            k.actf(hn[:], H[:, sub, :], AF.Square, [hr], ["hn", sr(0)], accum=S[:, c0:c0 + 1])
            k.ts("dve", S[:, c0 + 1:c0 + 2], S[:, c0:c0 + 1], 1.0 / D, EPS, ALU.mult, ALU.add, [sr(0)], [sr(1)])
            k.tt("pool", S[:, c0 + 2:c0 + 3], S[:, c0 + 1:c0 + 2], cmh[:], ALU.pow, [sr(1)], [sr(2)])
            k.stt(H[:, sub, :], H[:, sub, :], S[:, c0 + 2:c0 + 3], gfin[:], ALU.mult, ALU.mult, [hr, sr(2), "gfin"], [hr])
        k.dma("sp", "out%d" % b, out_d[i * 256:(i + 1) * 256, :].rearrange("(s p) d -> p s d", p=128), H[:],
              [("hb", b, 0), ("hb", b, 1)], [("out_d", i)])
    P.emit(nc)
    nc._prog_names = P.names
    return nc, sb


def _rope_table(L):
    half = 32
    inv = (np.float32(10000.0) ** (-(np.arange(half, dtype=np.float32) / np.float32(half)))).astype(np.float32)
    ang = np.arange(L, dtype=np.float32)[:, None] * inv[None, :]
    return np.concatenate([np.cos(ang), np.sin(ang)], axis=1).astype(np.float32)


def _cpar():
    kk = np.arange(128)
    return np.stack([(((kk % 16) % 2) == (c // 2)) for c in range(4)], axis=1).astype(np.float32)


def _csel2():
    kk = np.arange(128)[:, None]
    col = np.arange(64)[None, :]
    return (((kk // 16) == (col // 8)) & (((kk % 16) // 2) == (col % 8))).astype(np.float32)


def prep_inputs(inp, L):
    f = lambda a: np.ascontiguousarray(np.asarray(a, dtype=np.float32))
    g8 = lambda g: f(np.asarray(g).reshape(8, 128).T)
    shared = {
        "cs": _rope_table(L),
        "gA": g8(inp["g_attn"][0]), "gF": g8(inp["g_ffn"][0]), "gP": g8(inp["g_ple"][0]),
        "gfin": f(np.broadcast_to(np.asarray(inp["g_final"]).reshape(1, D), (128, D))),
        "gk": f(np.broadcast_to(np.asarray(inp["g_idx_k"][0]).reshape(1, 64), (128, 64))),
        "cpar": _cpar(), "csel2": _csel2(),
        "w_in": f(inp["w_in"][0]), "w_out": f(inp["w_out"][0]), "w_gate": f(inp["w_gate"][0]), "w_up": f(inp["w_up"][0]),
        "w_down": f(inp["w_down"][0]), "w_pg": f(inp["w_ple_gate"][0]), "w_pp": f(inp["w_ple_proj"][0]),
    }
    x = np.asarray(inp["x"], dtype=np.float32)
    p = np.asarray(inp["p"], dtype=np.float32)[0]
    return {"shared": shared, "x": [f(x[b]) for b in range(x.shape[0])], "p": [f(p[b]) for b in range(p.shape[0])]}


_NC_CACHE = {}


def kernel(**inputs):
    x = np.asarray(inputs["x"])
    Bn, L, _ = x.shape
    NT = L // 128
    prep = prep_inputs(inputs, L)
    if NT not in _NC_CACHE:
        _NC_CACHE[NT] = build(NT)[0]
    nc = _NC_CACHE[NT]
    in_maps = [dict(prep["shared"], x=prep["x"][b], p=prep["p"][b]) for b in range(Bn)]
    res = run_bass_kernel_spmd(nc, in_maps, core_ids=list(range(Bn)))
    return np.stack([np.asarray(res.results[b]["out"], dtype=np.float32) for b in range(Bn)], axis=0)
```

```python
import numpy as np
import concourse.bass as bass
import concourse.mybir as mybir
from concourse.bass_utils import run_bass_kernel_spmd

F32 = mybir.dt.float32
BF16 = mybir.dt.bfloat16
ALU = mybir.AluOpType
AF = mybir.ActivationFunctionType
AX = mybir.AxisListType

D = 1024
NIN = 2500
DFF = 2816
NFC = 22
DPLE = 256
C_QA, C_KA, C_VA, C_QI, C_KI, C_WI, C_QB, C_KB, C_VB = 0, 512, 576, 640, 896, 960, 964, 1476, 1988
S_QA, S_KA, S_QI, S_QB, S_KB, S_SG, QW = 0, 512, 576, 832, 1344, 1856, 1860
NEG = -30000.0
BIG = 1.0e30
KIT = 16
EPS = 1e-6
COMPUTE = ("pe", "act", "dve", "pool")
import os
WARMUP = int(os.environ.get("K_WARMUP", "20"))
WARMUP2 = int(os.environ.get("K_WARMUP2", "12"))


class Op:
    __slots__ = ("idx", "eng", "fn", "deps", "dma", "chan", "flag", "cnt", "semkey", "tag")

    def __init__(self, idx, eng, fn, deps, dma, chan):
        self.idx = idx
        self.eng = eng
        self.fn = fn
        self.deps = deps
        self.dma = dma
        self.chan = chan
        self.flag = False
        self.cnt = 0
        self.semkey = None


class Prog:
    def __init__(self):
        self.ops = []
        self.last_writer = {}
        self.readers = {}
        self.last_eng = {}
        self.last_chan = {}
        self.tag = ""
        self.names = {}

    def add(self, eng, fn, reads=(), writes=(), dma=False, chan=None, extra=()):
        idx = len(self.ops)
        deps = set(extra)
        for r in reads:
            w = self.last_writer.get(r)
            if w is not None:
                deps.add(w)
            if isinstance(r, tuple) and r[0] == "b":
                for rd in self.readers.get(r, ()):
                    if self.ops[rd].eng != eng:
                        deps.add(rd)
        for r in writes:
            w = self.last_writer.get(r)
            if w is not None:
                deps.add(w)
            for rd in self.readers.get(r, ()):
                deps.add(rd)
        for r in reads:
            self.readers.setdefault(r, []).append(idx)
        for r in writes:
            self.last_writer[r] = idx
            self.readers[r] = []
        deps.discard(idx)
        self.ops.append(Op(idx, eng, fn, deps, dma, chan))
        self.ops[-1].tag = self.tag
        if fn is not None:
            if dma:
                self.last_chan[chan] = idx
            else:
                self.last_eng[eng] = idx
        return idx

    def pe(self, fn, reads=(), writes=()):
        return self.add("pe", fn, reads, writes)

    def act(self, fn, reads=(), writes=()):
        return self.add("act", fn, reads, writes)

    def dve(self, fn, reads=(), writes=()):
        return self.add("dve", fn, reads, writes)

    def pool(self, fn, reads=(), writes=()):
        return self.add("pool", fn, reads, writes)

    def dma(self, eng, chan, fn, reads=(), writes=()):
        return self.add(eng, fn, reads, writes, dma=True, chan=chan)

    def barrier(self):
        deps = set(self.last_eng.values()) | set(self.last_chan.values())
        for e in ("sp", "pe", "act", "dve", "pool"):
            self.add(e, None, extra=deps)
        self.last_writer = {}
        self.readers = {}

    def emit(self, nc, final_wait_eng="sp"):
        ops = self.ops
        for op in ops:
            last = {}
            for d in op.deps:
                p = ops[d]
                if p.dma:
                    p.flag = True
                elif op.dma or p.eng != op.eng or p.eng in ("act", "dve", "pool"):
                    if last.get(p.eng, -1) < d:
                        last[p.eng] = d
            for d in last.values():
                ops[d].flag = True
        chans = {}
        engs = {}
        for op in ops:
            if op.fn is None:
                continue
            if op.dma:
                op.flag = True
                c = chans.get(op.chan, 0) + 16
                chans[op.chan] = c
                op.cnt = c
                op.semkey = ("dma", op.chan)
            else:
                if op.flag:
                    c = engs.get(op.eng, 0) + 1
                    engs[op.eng] = c
                    op.cnt = c
                op.semkey = ("eng", op.eng)
        semkeys = [("eng", e) for e in COMPUTE] + [("dma", c) for c in chans]
        self.nsems = len(semkeys)
        streams = {}
        for op in ops:
            streams.setdefault(op.eng, []).append(op)
        sems = {}
        for k in semkeys:
            sems[k] = nc.alloc_semaphore(name=("s_%s_%s" % k))
        attr = {"pe": "tensor", "act": "scalar", "dve": "vector", "pool": "gpsimd", "sp": "sync"}
        with nc.Block() as block0:

            def clear_body(eng):
                for k in semkeys:
                    eng.sem_clear(sems[k])

            block0.sync(clear_body)
        with nc.Block() as block:

            def make_body(elist, is_final):
                def body(eng):
                    waited = {}
                    for op in elist:
                        need = {}
                        lastd = {}
                        for d in op.deps:
                            p = ops[d]
                            if p.dma:
                                lastd[("c", d)] = d
                            elif lastd.get(p.eng, -1) < d:
                                lastd[p.eng] = d
                        for d in lastd.values():
                            p = ops[d]
                            if not p.flag:
                                continue
                            if (not p.dma) and (not op.dma) and p.eng == op.eng and op.eng == "pe":
                                continue
                            k = p.semkey
                            if need.get(k, 0) < p.cnt:
                                need[k] = p.cnt
                        for k, v in need.items():
                            if waited.get(k, 0) >= v:
                                continue
                            eng.wait_ge(sems[k], v)
                            waited[k] = v
                        if op.fn is None:
                            continue
                        ins = op.fn(eng)
                        try:
                            self.names[ins.ins.name] = op.tag
                        except Exception:
                            pass
                        if op.flag:
                            ins.then_inc(sems[op.semkey], 16 if op.dma else 1)
                    if is_final:
                        for c, v in chans.items():
                            if waited.get(("dma", c), 0) < v:
                                eng.wait_ge(sems[("dma", c)], v)
                        for e, v in engs.items():
                            if v > 0 and waited.get(("eng", e), 0) < v:
                                eng.wait_ge(sems[("eng", e)], v)

                return body

            for ename in ("sp", "pe", "act", "dve", "pool"):
                elist = streams.get(ename, [])
                is_final = ename == final_wait_eng
                if not elist and not is_final:
                    continue
                getattr(block, attr[ename])(make_body(elist, is_final))
        return nc


class SB:
    def __init__(self, nc, base=16512, top=229344):
        self.nc = nc
        self.off = base
        self.top = top
        self.n = 0
        self.peak = base

    def alloc(self, name, shape, dt):
        esz = 4 if dt == F32 else 2
        nb = esz
        for s in shape[1:]:
            nb *= s
        self.n += 1
        t = self.nc.alloc_sbuf_tensor_at("%s_%d" % (name, self.n), list(shape), dt, offset=self.off)
        self.off += (nb + 31) // 32 * 32
        assert self.off <= self.top, "SBUF overflow at %s: %d > %d" % (name, self.off, self.top)
        self.peak = max(self.peak, self.off)
        return t

    def mark(self):
        return self.off

    def reset(self, m):
        self.off = m


class KB:
    def __init__(self, P):
        self.P = P

    def mm(self, out, lhsT, rhs, start, stop, reads, writes, skip=False):
        self.P.pe(lambda e: e.matmul(out, lhsT=lhsT, rhs=rhs, start=start, stop=stop, skip_group_check=skip), reads, writes)

    def tr(self, out, in_, ident, reads, writes):
        self.P.pe(lambda e: e.transpose(out, in_, ident), reads, writes)

    def actf(self, out, in_, func, reads, writes, scale=None, bias=None, accum=None):
        kw = {}
        if scale is not None:
            kw["scale"] = scale
        if bias is not None:
            kw["bias"] = bias
        if accum is not None:
            kw["accum_out"] = accum
        self.P.act(lambda e: e.activation(out=out, in_=in_, func=func, **kw), reads, writes)

    def ts(self, eng, out, in0, s1, s2, op0, op1, reads, writes, accum=None):
        kw = {}
        if op1 is not None:
            kw["op1"] = op1
        if accum is not None:
            kw["accum_out"] = accum
        self.P.add(eng, lambda e: e.tensor_scalar(out=out, in0=in0, scalar1=s1, scalar2=s2, op0=op0, **kw), reads, writes)

    def tt(self, eng, out, in0, in1, op, reads, writes):
        self.P.add(eng, lambda e: e.tensor_tensor(out=out, in0=in0, in1=in1, op=op), reads, writes)

    def stt(self, out, in0, scalar, in1, op0, op1, reads, writes):
        self.P.dve(lambda e: e.scalar_tensor_tensor(out=out, in0=in0, scalar=scalar, in1=in1, op0=op0, op1=op1), reads, writes)

    def cp(self, eng, out, in_, reads, writes):
        self.P.add(eng, lambda e: e.tensor_copy(out, in_), reads, writes)

    def memset(self, eng, ap, val, reads, writes):
        self.P.add(eng, lambda e: e.memset(ap, val), reads, writes)

    def dma(self, eng, chan, out, in_, reads, writes):
        self.P.dma(eng, chan, lambda e: e.dma_start(out=out, in_=in_), reads, writes)


def build(NT, stage=3, debug=False):
    L = NT * 128
    nc = bass.Bass("TRN2", target_bir_lowering=False)

    def din(name, shape):
        return nc.dram_tensor(name, list(shape), F32, kind="ExternalInput").ap()

    x_d = din("x", [L, D])
    p_d = din("p", [L, DPLE])
    cs_d = din("cs", [L, 64])
    gA_d = din("gA", [128, 8])
    gF_d = din("gF", [128, 8])
    gP_d = din("gP", [128, 8])
    gfin_d = din("gfin", [128, D])
    gk_d = din("gk", [128, 64])
    par_d = din("cpar", [128, 4])
    sel2_d = din("csel2", [128, 64])
    w_in_d = din("w_in", [D, NIN])
    w_out_d = din("w_out", [D, D])
    w_gate_d = din("w_gate", [D, DFF])
    w_up_d = din("w_up", [D, DFF])
    w_down_d = din("w_down", [DFF, D])
    w_pg_d = din("w_pg", [D, D])
    w_pp_d = din("w_pp", [DPLE, D])
    out_d = nc.dram_tensor("out", [L, D], F32, kind="ExternalOutput").ap()
    skind = "ExternalOutput" if debug else "Internal"
    qs_d = nc.dram_tensor("qs", [L, QW], BF16, kind=skind).ap()
    h1_d = nc.dram_tensor("h1", [L, D], F32, kind=skind).ap()
    dbg = {}
    if debug:
        dbg["kaT"] = nc.dram_tensor("d_kaT", [128, L], BF16, kind="ExternalOutput").ap()
        dbg["kiT"] = nc.dram_tensor("d_kiT", [128, L], BF16, kind="ExternalOutput").ap()
        dbg["kbT"] = nc.dram_tensor("d_kbT", [128, 4, L], BF16, kind="ExternalOutput").ap()
        dbg["va1"] = nc.dram_tensor("d_va1", [128, NT, 65], BF16, kind="ExternalOutput").ap()
        dbg["vb1"] = nc.dram_tensor("d_vb1", [128, NT, 8, 65], BF16, kind="ExternalOutput").ap()
        dbg["kmT"] = nc.dram_tensor("d_kmT", [128, 4, 16], BF16, kind="ExternalOutput").ap()
        dbg["y"] = nc.dram_tensor("d_y", [L, D], BF16, kind="ExternalOutput").ap()
        dbg["kaT_end"] = nc.dram_tensor("d_kaT_end", [128, L], BF16, kind="ExternalOutput").ap()

    sb = SB(nc)
    P = Prog()
    k = KB(P)
    banks = [nc.alloc_psum_tensor("bank%d" % i, [128, 512], F32) for i in range(8)]
    bankb = [b[:, :].bitcast(BF16) for b in banks]

    def B(i):
        return ("b", i)

    ident = sb.alloc("ident", [128, 128], F32)
    identb = sb.alloc("identb", [128, 128], BF16)
    ident4 = sb.alloc("ident4", [128, 4, 128], BF16)
    tri = sb.alloc("tri", [128, 128], BF16)
    cpow = sb.alloc("cpow", [128, KIT + 2], F32)
    cm1 = sb.alloc("cm1", [128, 8, 1], F32)
    cmh = sb.alloc("cmh", [128, 1], F32)
    k.memset("pool", ident[:], 0.0, [], ["ident"])
    P.pool(lambda e: e.affine_select(out=ident[:], in_=ident[:], pattern=[[-1, 128]], compare_op=ALU.not_equal, fill=1.0,
                                     base=0, channel_multiplier=1), ["ident"], ["ident"])
    k.cp("dve", identb[:], ident[:], ["ident"], ["identb"])
    k.cp("dve", ident4[:], identb[:].unsqueeze(1).to_broadcast([128, 4, 128]), ["identb"], ["ident4"])
    k.memset("pool", tri[:], 0.0, [], ["tri"])
    P.pool(lambda e: e.affine_select(out=tri[:], in_=tri[:], pattern=[[1, 128]], compare_op=ALU.is_ge, fill=NEG,
                                     base=0, channel_multiplier=-1), ["tri"], ["tri"])
    for kk in range(KIT + 2):
        k.memset("pool", cpow[:, kk:kk + 1], float(2.0 ** (-(kk - 1))), [], ["cpow"])
    k.memset("pool", cm1[:], -1.0, [], ["cm1"])
    par = sb.alloc("par", [128, 4], F32)
    sel2f = sb.alloc("sel2f", [128, 64], F32)
    sel2 = sb.alloc("sel2", [128, 64], BF16)
    nsz = sb.alloc("nsz", [128, 4, 128], BF16)
    k.dma("sp", "par", par[:], par_d, [], ["par"])
    k.dma("sp", "sel2", sel2f[:], sel2_d, [], ["sel2f"])
    k.cp("dve", sel2[:], sel2f[:], ["sel2f"], ["sel2"])
    k.memset("pool", nsz[:], 0.0, [], ["nsz"])
    k.memset("pool", cmh[:], -0.5, [], ["cmh"])

    mark_glob = sb.mark()
    kaT2 = sb.alloc("kaT2", [128, L], BF16)
    kiT2 = sb.alloc("kiT2", [128, L], BF16)
    kbT = sb.alloc("kbT", [128, 4, L], BF16)
    va1 = sb.alloc("va1", [128, NT, 65], BF16)
    vb1 = sb.alloc("vb1", [128, NT, 8, 65], BF16)
    kmT = sb.alloc("kmT", [128, 4, 16], BF16)
    mark_phase = sb.mark()

    w_in = sb.alloc("w_in", [128, 8, NIN], BF16)
    gA = sb.alloc("gA", [128, 8], F32)
    gk = sb.alloc("gk", [128, 64], F32)
    xin = [sb.alloc("xin", [128, D], F32) for _ in range(2)]
    cst = [sb.alloc("cst", [128, 64], F32) for _ in range(2)]
    xT = sb.alloc("xT", [128, 8, 128], BF16)
    proj = [sb.alloc("proj", [128, NIN], F32) for _ in range(2)]
    rtmp = [sb.alloc("rtmp", [128, 30 * 32], F32) for _ in range(4)]
    qif = sb.alloc("qif", [128, 256], F32)
    kct = sb.alloc("kct", [128, 64], F32)
    knt = sb.alloc("knt", [128, 64], F32)
    ki1 = sb.alloc("ki1", [128, 64], BF16)
    ka2s = [sb.alloc("ka2", [128, 128], BF16) for _ in range(2)]
    ki2s = [sb.alloc("ki2", [128, 128], BF16) for _ in range(2)]
    qst = [sb.alloc("qst", [128, QW], BF16) for _ in range(2)]
    junkx = sb.alloc("junkx", [128, D], BF16)
    st = [sb.alloc("st", [128, 16], F32) for _ in range(2)]
    kmf = sb.alloc("kmf", [128, 4], F32)

    for hf in range(2):
        k.dma("pool", "win%d" % hf, w_in[:, hf * 4:(hf + 1) * 4, :],
              w_in_d[hf * 512:(hf + 1) * 512, :].rearrange("(c p) n -> p c n", p=128), [], [("w_in", hf)])
    k.dma("sp", "gA", gA[:], gA_d, [], ["gA"])
    k.dma("sp", "gk", gk[:], gk_d, [], ["gk"])
    k.memset("pool", va1[:], 1.0, [], ["va1init"])
    k.memset("pool", vb1[:], 1.0, [], ["vb1init"])
    k.memset("pool", kmT[:], 0.0, [], ["kmT"])

    def rope(src, dst, H, t0, cs, rsrc, wdst, tag):
        def v(ap):
            return ap.rearrange("p (h t d) -> p h t d", t=2, d=32)

        x1 = v(src)[:, :, 0, :]
        x2 = v(src)[:, :, 1, :]
        d1 = v(dst)[:, :, 0, :]
        d2 = v(dst)[:, :, 1, :]
        cos = cs[:, 0:32].unsqueeze(1).to_broadcast([128, H, 32])
        sin = cs[:, 32:64].unsqueeze(1).to_broadcast([128, H, 32])
        tv = [t[:, t0 * 32:(t0 + H) * 32].rearrange("p (h d) -> p h d", d=32) for t in rtmp]
        rn = [("rt", n, tag) for n in range(4)]
        k.tt("dve", tv[0], x1, cos, ALU.mult, rsrc, [rn[0]])
        k.tt("pool", tv[1], x2, sin, ALU.mult, rsrc, [rn[1]])
        k.tt("dve", d1, tv[0], tv[1], ALU.subtract, [rn[0], rn[1]], wdst)
        k.tt("pool", tv[2], x2, cos, ALU.mult, rsrc, [rn[2]])
        k.tt("dve", tv[3], x1, sin, ALU.mult, rsrc, [rn[3]])
        k.tt("pool", d2, tv[2], tv[3], ALU.add, [rn[2], rn[3]], wdst)

    def a0_front(i):
        b = i % 2
        r0, r1 = i * 128, (i + 1) * 128
        P.tag = "A0.stats"
        k.dma("sp", "x%d" % b, xin[b][:], x_d[r0:r1, :], [], [("xin", b)])
        k.dma("sp", "cs%d" % b, cst[b][:], cs_d[r0:r1, :], [], [("cst", b)])
        S = st[b]
        sr = lambda c: ("st", b, c)
        k.actf(junkx[:], xin[b][:], AF.Square, [("xin", b)], ["junkx", sr(0)], accum=S[:, 0:1])
        k.ts("dve", S[:, 1:2], S[:, 0:1], 1.0 / D, EPS, ALU.mult, ALU.add, [sr(0)], [sr(1)])
        k.tt("pool", S[:, 2:3], S[:, 1:2], cmh[:], ALU.pow, [sr(1), "cmh"], [sr(2)])
        P.tag = "A0.xT"
        for kc in range(8):
            k.tr(banks[kc // 4][:, (kc % 4) * 128:(kc % 4 + 1) * 128], xin[b][:, kc * 128:(kc + 1) * 128], ident[:],
                 [("xin", b), "ident"], [B(kc // 4)])
        for hf in range(2):
            k.tt("dve", xT[:, hf * 4:(hf + 1) * 4, :], banks[hf][:, :].rearrange("p (c t) -> p c t", c=4),
                 gA[:, hf * 4:(hf + 1) * 4].unsqueeze(2).to_broadcast([128, 4, 128]), ALU.mult, [B(hf), "gA"], [("xT", hf)])
        P.tag = "A0.inproj"
        pj = proj[b]
        for nb in range(5):
            n0 = nb * 512
            n1 = min(NIN, n0 + 512)
            w = n1 - n0
            for kc in range(8):
                k.mm(banks[2 + nb][:, 0:w], xT[:, kc, :], w_in[:, kc, n0:n1], kc == 0, kc == 7,
                     [("xT", kc // 4), ("w_in", kc // 4)], [B(2 + nb)])
            k.actf(pj[:, n0:n1], banks[2 + nb][:, 0:w], AF.Copy, [B(2 + nb), sr(2)], [("proj", b, nb)], scale=S[:, 2:3])
    def a0_back(i):
        b = i % 2
        r0, r1 = i * 128, (i + 1) * 128
        S = st[b]
        sr = lambda c: ("st", b, c)
        pj = proj[b]
        P.tag = "A0.rope"
        pall = [("proj", b, nb) for nb in range(5)]
        Q = qst[b]
        qr = ("qst", b)
        ka2, ki2 = ka2s[b], ki2s[b]
        rope(pj[:, 0:576], Q[:, 0:576], 9, 0, cst[b], pall[0:2] + [("cst", b)], [qr], "a")
        k.cp("pool", ka2[:].rearrange("p (c d) -> p c d", c=2), Q[:, S_KA:S_KA + 64].unsqueeze(1).to_broadcast([128, 2, 64]),
             [qr], [("ka2", b)])
        rope(pj[:, C_QB:C_QB + 1024], Q[:, S_QB:S_QB + 1024], 16, 9, cst[b], pall[1:4] + [("cst", b)], [qr], "b")
        k.actf(S[:, 8:12], pj[:, C_WI:C_WI + 4], AF.Abs, [pall[1]], [sr(8)], scale=0.125)
        k.ts("dve", Q[:, S_SG:S_SG + 4], pj[:, C_WI:C_WI + 4], 0.0, -0.5, ALU.is_ge, ALU.add, [pall[1]], [qr])
        rope(pj[:, C_QI:C_QI + 256], qif[:], 4, 25, cst[b], [pall[1], ("cst", b)], ["qif"], "i")
        k.tt("dve", Q[:, S_QI:S_QI + 256].rearrange("p (h d) -> p h d", h=4), qif[:].rearrange("p (h d) -> p h d", h=4),
             S[:, 8:12].unsqueeze(2).to_broadcast([128, 4, 64]), ALU.mult, ["qif", sr(8)], [qr])
        P.dve(lambda e, S=S, pj=pj: e.tensor_reduce(out=S[:, 3:4], in_=pj[:, C_KI:C_KI + 64], axis=AX.X, op=ALU.add),
              [pall[1]], [sr(3)])
        k.ts("dve", S[:, 4:5], S[:, 3:4], -1.0 / 64, None, ALU.mult, None, [sr(3)], [sr(4)])
        k.ts("dve", kct[:], pj[:, C_KI:C_KI + 64], S[:, 4:5], None, ALU.add, None, [pall[1], sr(4)], ["kct"])
        k.actf(junkx[:, 0:64], kct[:], AF.Square, ["kct"], ["junkx", sr(5)], accum=S[:, 5:6])
        k.ts("dve", S[:, 6:7], S[:, 5:6], 1.0 / 64, EPS, ALU.mult, ALU.add, [sr(5)], [sr(6)])
        k.tt("pool", S[:, 7:8], S[:, 6:7], cmh[:], ALU.pow, [sr(6), "cmh"], [sr(7)])
        k.stt(knt[:], kct[:], S[:, 7:8], gk[:], ALU.mult, ALU.mult, ["kct", sr(7), "gk"], ["knt"])
        rope(knt[:], ki1[:], 1, 29, cst[b], ["knt", ("cst", b)], ["ki1"], "k")
        k.cp("pool", ki2[:].rearrange("p (c d) -> p c d", c=2), ki1[:].unsqueeze(1).to_broadcast([128, 2, 64]), ["ki1"], [("ki2", b)])
        P.tag = "A0.kT"
        k.actf(va1[:, i, 0:64], pj[:, C_VA:C_VA + 64], AF.Copy, [pall[1], "va1init"], [("va1", i)])
        k.actf(vb1[:, i, :, 0:64], pj[:, C_VB:C_VB + 512].rearrange("p (h d) -> p h d", h=8), AF.Copy,
               [pall[3], pall[4], "vb1init"], [("vb1", i)])
    def a0_T(i):
        b = i % 2
        r0, r1 = i * 128, (i + 1) * 128
        Q = qst[b]
        qr = ("qst", b)
        ka2, ki2 = ka2s[b], ki2s[b]
        P.tag = "A0.kT"
        tp = bankb[7]
        k.tr(tp[:, 0:128], ka2[:], identb[:], [("ka2", b), "identb"], [B(7)])
        k.tr(tp[:, 128:256], ki2[:], identb[:], [("ki2", b), "identb"], [B(7)])
        for pp in range(4):
            k.tr(tp[:, 256 + pp * 128:384 + pp * 128], Q[:, S_KB + pp * 128:S_KB + (pp + 1) * 128], identb[:], [qr, "identb"], [B(7)])
        k.actf(kaT2[:, r0:r1], tp[:, 0:128], AF.Copy, [B(7)], [("kaT", i)])
        k.cp("dve", kiT2[:, r0:r1], tp[:, 128:256], [B(7)], [("kiT", i)])
        k.actf(kbT[:, :, r0:r1], tp[:, 256:768].rearrange("p (c t) -> p c t", c=4), AF.Copy, [B(7)], [("kbT", i)])
        if i % 2 == 1:
            j = i // 2
            P.dve(lambda e, j=j: e.tensor_reduce(out=kmf[:], in_=kbT[:, :, j * 256:(j + 1) * 256], axis=AX.X, op=ALU.add),
                  [("kbT", i - 1), ("kbT", i)], ["kmf"])
            k.ts("dve", kmT[:, :, j], kmf[:], 1.0 / 256, None, ALU.mult, None, ["kmf", "kmT"], ["kmT"])
        k.dma("sp", "qs%d" % b, qs_d[r0:r1, :], Q[:], [qr], [("qs_d", i)])

    a0_front(0)
    for i in range(NT):
        if i + 1 < NT:
            a0_front(i + 1)
        a0_back(i)
        if i >= 1:
            a0_T(i - 1)
    a0_T(NT - 1)

    if debug:
        P.barrier()
        k.dma("sp", "dbg", dbg["kaT"], kaT2[:], [], [])
        k.dma("sp", "dbg", dbg["kiT"], kiT2[:], [], [])
        k.dma("sp", "dbg", dbg["kbT"], kbT[:], [], [])
        k.dma("sp", "dbg", dbg["va1"], va1[:], [], [])
        k.dma("sp", "dbg", dbg["vb1"], vb1[:], [], [])
        k.dma("sp", "dbg", dbg["kmT"], kmT[:], [], [])
    if stage < 2:
        P.emit(nc)
        return nc, sb

    P.barrier()
    sb.reset(mark_phase)
    w_out = sb.alloc("w_out", [128, 8, D], BF16)
    qin = [sb.alloc("qin", [128, QW], BF16) for _ in range(2)]
    xin = [sb.alloc("xin1", [128, D], F32) for _ in range(2)]
    qaT = [sb.alloc("qaT", [64, 1024], BF16) for _ in range(2)]
    qbT = [sb.alloc("qbT", [128, 4, 128], BF16) for _ in range(2)]
    qiT = sb.alloc("qiT", [64, 512], BF16)
    dgs = sb.alloc("dgs", [128, 4, 128], BF16)
    rl = [sb.alloc("rl", [128, 4, 256], BF16) for _ in range(2)]
    sc = sb.alloc("sc", [128, L], F32)
    junk = sb.alloc("junk", [128, L], BF16)
    mb = [sb.alloc("mb", [128, L], BF16) for _ in range(2)]
    sst = [sb.alloc("sst", [128, 40], F32) for _ in range(2)]
    steps2 = sb.alloc("steps2", [128, KIT + 2], F32)
    mids = sb.alloc("mids", [128, KIT + 2], F32)
    cnts = sb.alloc("cnts", [128, KIT + 2], F32)
    sks = sb.alloc("sks", [128, KIT + 2], F32)
    gate = sb.alloc("gate", [128, 8, 16], F32)
    top8 = sb.alloc("top8", [128, 8, 8], F32)
    nsf = sb.alloc("nsf", [128, 8, 16], F32)
    negsel = sb.alloc("negsel", [128, 128], BF16)
    NS4 = [sb.alloc("NS4", [128, 4, 128], BF16) for _ in range(2)]
    PTe = [sb.alloc("PTe", [128, 512], BF16) for _ in range(2)]
    PTo = [sb.alloc("PTo", [128, 512], BF16) for _ in range(2)]
    ysb = [sb.alloc("ysb", [128, 8, 65], F32) for _ in range(2)]
    rec = [sb.alloc("rec", [128, 8, 1], F32) for _ in range(2)]
    yt = sb.alloc("y", [128, D], BF16)
    yT = sb.alloc("yT", [128, 8, 128], BF16)
    tmp = sb.alloc("tmp", [128, D], F32)
    h1t = [sb.alloc("h1t", [128, D], F32) for _ in range(2)]

    k.dma("pool", "wout", w_out[:], w_out_d.rearrange("(c p) n -> p c n", p=128), [], ["w_out"])

    def a1_loads(i):
        b = i % 2
        k.dma("sp", "q%d" % b, qin[b][:], qs_d[i * 128:(i + 1) * 128, :], [], [("qin", b)])
        k.dma("sp", "x1%d" % b, xin[b][:], x_d[i * 128:(i + 1) * 128, :], [], [("xin", b)])

    def a1_front(i):
        b = i % 2
        j = i // 2
        Lk = (i + 1) * 128
        Qn = qin[b]
        qr = ("qin", b)
        P.tag = "A1.T"
        for h in range(8):
            k.tr(bankb[6][0:64, h * 128:(h + 1) * 128], Qn[:, S_QA + h * 64:S_QA + (h + 1) * 64], identb[:], [qr], [B(6)])
        for pp in range(4):
            k.tr(bankb[7][:, pp * 128:(pp + 1) * 128], Qn[:, S_QB + pp * 128:S_QB + (pp + 1) * 128], identb[:], [qr], [B(7)])
        for h in range(4):
            k.tr(bankb[7][0:64, 512 + h * 128:512 + (h + 1) * 128], Qn[:, S_QI + h * 64:S_QI + (h + 1) * 64], identb[:], [qr], [B(7)])
        k.actf(qaT[b][:], bankb[6][0:64, :], AF.Copy, [B(6)], [("qaT", b)])
        k.actf(qbT[b][:], bankb[7][:, 0:512].rearrange("p (c t) -> p c t", c=4), AF.Copy, [B(7)], [("qbT", b)])
        k.actf(qiT[:], bankb[7][0:64, 512:1024], AF.Copy, [B(7)], ["qiT"])
        for h in range(4):
            k.ts("dve", dgs[:, h, :], identb[:], Qn[:, S_SG + h:S_SG + h + 1], None, ALU.mult, None, [qr], [("dgs", h)])
        if j >= 1:
            P.tag = "A1.gate"
            for g in range(2):
                P.act(lambda e, g=g: e.memzero(banks[6 + g][:, 0:64]), [], [B(6 + g)])
            for g in range(2):
                for pp in range(4):
                    k.mm(banks[6 + g][:, pp * 16:(pp + 1) * 16], qbT[b][g * 64:(g + 1) * 64, pp, :], kmT[g * 64:(g + 1) * 64, pp, :],
                         False, False, [("qbT", b)], [B(6 + g)], skip=True)
            for g in range(2):
                k.actf(gate[:, g * 4:(g + 1) * 4, :], banks[6 + g][:, 0:64].rearrange("p (c n) -> p c n", c=4), AF.Copy,
                       [B(6 + g)], ["gate"])
            if j < 16:
                k.memset("pool", gate[:, :, j:16], -BIG, ["gate"], ["gate"])
            for hh in range(8):
                P.dve(lambda e, hh=hh: e.max(out=top8[:, hh, :], in_=gate[:, hh, :]), ["gate"], [("top8", hh)])
            k.tt("dve", nsf[:], gate[:], top8[:, :, 2:3].to_broadcast([128, 8, 16]), ALU.is_lt,
                 ["gate"] + [("top8", hh) for hh in range(8)], ["nsf"])
            if j < 16:
                k.memset("dve", nsf[:, :, j:16], 0.0, ["nsf"], ["nsf"])
            k.ts("dve", negsel[:], nsf[:].rearrange("p h n -> p (h n)"), NEG, None, ALU.mult, None, ["nsf"], ["negsel"])
        if WARMUP2 > 0:
            P.tag = "A1.warm"
            for wq in range(WARMUP2):
                k.mm(banks[4 + wq % 2][:, :], ident4[:, wq % 4, :], ident4[:], True, True, [], [B(4 + wq % 2)])
        P.tag = "A1.idx"
        SS = sst[b]
        sr = ("sst", b)
        nch = (Lk + 255) // 256

        def Zc(c):
            c0 = c * 256
            w = min(256, Lk - c0)
            st_ = c % 2
            for h in range(4):
                bk = st_ * 2 + h // 2
                col = (h % 2) * 256
                k.mm(banks[bk][:, col:col + w], qiT[:, h * 128:(h + 1) * 128], kiT2[0:64, c0:c0 + w], True, True,
                     ["qiT"], [B(bk)], skip=True)
            for hb in range(2):
                bk = st_ * 2 + hb
                k.actf(rl[st_][:, hb * 2:(hb + 1) * 2, 0:w], banks[bk][:, :].rearrange("p (h x) -> p h x", h=2)[:, :, 0:w], AF.Relu,
                       [B(bk)], [("rl", st_, hb)])

        def SSc(c):
            c0 = c * 256
            w = min(256, Lk - c0)
            st_ = c % 2
            sbk = 6 + c % 2
            for h in range(4):
                k.mm(banks[sbk][:, 0:w], dgs[:, h, :], rl[st_][:, h, 0:w], h == 0, h == 3, [("dgs", h), ("rl", st_, h // 2)], [B(sbk)])
            k.ts("dve", sc[:, c0:c0 + w], banks[sbk][:, 0:w], 1.0, -3.0e38, ALU.mult, ALU.max, [B(sbk)], ["sc", sr],
                 accum=SS[:, c:c + 1])
            k.ts("dve", junk[:, c0:c0 + w], sc[:, c0:c0 + w], 1.0, 3.0e38, ALU.mult, ALU.min, ["sc"], ["junk", sr],
                 accum=SS[:, 16 + c:17 + c])

        Zc(0)
        for c in range(1, nch):
            Zc(c)
            SSc(c - 1)
        SSc(nch - 1)
        if j >= 1:
            P.tag = "A1.gate"
            k.tr(bankb[7][:, 0:128], negsel[:], identb[:], ["negsel"], [B(7)])
            k.tt("dve", NS4[b][:], bankb[7][:, 0:128].unsqueeze(1).to_broadcast([128, 4, 128]),
                 par[:].unsqueeze(2).to_broadcast([128, 4, 128]), ALU.mult, [B(7), "par"], [("NS4", b)])
        P.tag = "A1.search"
        if i >= 2:
            if nch > 1:
                P.dve(lambda e: e.tensor_reduce(out=SS[:, 32:33], in_=SS[:, 0:nch], axis=AX.X, op=ALU.max), [sr], [sr])
                P.dve(lambda e: e.tensor_reduce(out=SS[:, 33:34], in_=SS[:, 16:16 + nch], axis=AX.X, op=ALU.min), [sr], [sr])
                cmax, cmin = SS[:, 32:33], SS[:, 33:34]
            else:
                cmax, cmin = SS[:, 0:1], SS[:, 16:17]
            k.tt("dve", SS[:, 34:35], cmax, cmin, ALU.subtract, [sr], [sr])
            k.ts("dve", steps2[:], cpow[:], SS[:, 34:35], None, ALU.mult, None, [sr, "cpow"], ["steps2"])
            k.stt(mids[:, 1:2], SS[:, 34:35], 0.5, cmin, ALU.mult, ALU.add, [sr], ["mids"])
        P.pool(lambda e: e.affine_select(out=sc[:, i * 128:(i + 1) * 128], in_=sc[:, i * 128:(i + 1) * 128], pattern=[[-1, 128]],
                                         compare_op=ALU.is_ge, fill=-BIG, base=0, channel_multiplier=1), ["sc"], ["sc"])
        if i >= 2:
            for kk in range(1, KIT + 1):
                k.ts("dve", junk[:, 0:Lk], sc[:, 0:Lk], mids[:, kk:kk + 1], 0.0, ALU.is_ge, ALU.add, ["sc", "mids"], ["junk", "cnts"],
                     accum=cnts[:, kk:kk + 1])
                k.ts("dve", sks[:, kk:kk + 1], cnts[:, kk:kk + 1], 255.5, -0.5, ALU.is_ge, ALU.add, ["cnts"], ["sks"])
                k.stt(mids[:, kk + 1:kk + 2], sks[:, kk:kk + 1], steps2[:, kk + 1:kk + 2], mids[:, kk:kk + 1], ALU.mult, ALU.add,
                      ["sks", "steps2", "mids"], ["mids"])
            k.stt(SS[:, 35:36], steps2[:, KIT + 1:KIT + 2], -0.5, mids[:, KIT + 1:KIT + 2], ALU.mult, ALU.add, ["steps2", "mids"], [sr])
        else:
            k.memset("dve", SS[:, 35:36], -1.0e29, [sr], [sr])
        P.tag = "A1.mask"
        k.ts("dve", mb[b][:, 0:Lk], sc[:, 0:Lk], SS[:, 35:36], NEG, ALU.is_lt, ALU.mult, ["sc", sr], [("mb", b)])

    cc_state = [0]

    def a1_back(i):
        b = i % 2
        j = i // 2
        nkt = i + 1
        YM = [banks[4], banks[5]]
        YD = [banks[6], banks[7]]
        ystart_m = [True, True]
        ystart_d = [True, True]
        steps = []
        for pp in range(4):
            for c in range((nkt + 3) // 4):
                steps.append(("m", pp, c))
        n_moba = len(steps)
        for kt in range(nkt):
            steps.append(("d", kt, 0))

        def S(step, s):
            kind, a0, a1 = step
            if kind == "m":
                pp, c = a0, a1
                E, O = banks[s], banks[2 + s]
                kts = list(range(4 * c, min(4 * c + 4, nkt)))
                w = len(kts) * 128
                rhs_ns = (NS4[b] if j >= 1 else nsz)[:, 0:len(kts), :]
                rd_ns = [("NS4", b)] if j >= 1 else []
                for (bank, bi, hh, g) in ((E, s, pp, 0), (O, 2 + s, 4 + pp, 1)):
                    P.tag = "A1.moba.bias"
                    k.mm(bank[:, 0:w], sel2[:, hh * 8 + c:hh * 8 + c + 1].to_broadcast([128, 128]), rhs_ns, True, False,
                         rd_ns, [B(bi)], skip=True)
                    P.tag = "A1.moba.s"
                    for kt in kts:
                        col = (kt % 4) * 128
                        k.mm(bank[:, col:col + 128], kbT[g * 64:(g + 1) * 64, pp, kt * 128:(kt + 1) * 128],
                             qbT[b][g * 64:(g + 1) * 64, pp, :], False, False, [("qbT", b)], [B(bi)], skip=True)
                        if kt == i:
                            k.mm(bank[:, col:col + 128], identb[:], tri[:], False, False, [], [B(bi)], skip=True)
                P.tag = "A1.moba.exp"
                k.actf(PTe[s][:, 0:w], E[:, 0:w], AF.Exp, [B(s)], [("PTe", s)], scale=0.125)
                k.actf(PTo[s][:, 0:w], O[:, 0:w], AF.Exp, [B(2 + s)], [("PTo", s)], scale=0.125)
            else:
                kt = a0
                SA, SBk = banks[s], banks[2 + s]
                ks = slice(kt * 128, (kt + 1) * 128)
                P.tag = "A1.dsa.s"
                k.mm(SA[:, :], kaT2[0:64, ks], qaT[b][:, 0:512], True, False, [("qaT", b)], [B(s)])
                P.tag = "A1.dsa.mask"
                k.mm(SA[:, :], mb[b][:, ks], ident4[:], False, True, [("mb", b)], [B(s)])
                P.tag = "A1.dsa.s"
                k.mm(SBk[:, :], kaT2[0:64, ks], qaT[b][:, 512:1024], True, False, [("qaT", b)], [B(2 + s)])
                P.tag = "A1.dsa.mask"
                k.mm(SBk[:, :], mb[b][:, ks], ident4[:], False, True, [("mb", b)], [B(2 + s)])
                P.tag = "A1.dsa.exp"
                k.actf(PTe[s][:], SA[:, :], AF.Exp, [B(s)], [("PTe", s)], scale=0.125)
                k.actf(PTo[s][:], SBk[:, :], AF.Exp, [B(2 + s)], [("PTo", s)], scale=0.125)

        def V(step, s):
            kind, a0, a1 = step
            if kind == "m":
                pp, c = a0, a1
                he, ho = 2 * pp, 2 * pp + 1
                kts = list(range(4 * c, min(4 * c + 4, nkt)))
                P.tag = "A1.moba.pv"
                for kt in kts:
                    col = (kt % 4) * 128
                    for (PT, pn, h) in ((PTe[s], ("PTe", s), he), (PTo[s], ("PTo", s), ho)):
                        yb_ = h // 4
                        k.mm(YM[yb_][:, (h % 4) * 65:(h % 4) * 65 + 65], PT[:, col:col + 128], vb1[:, kt, h, :], False, False,
                             [pn], [B(4 + yb_)], skip=True)
                        ystart_m[yb_] = False
            else:
                kt = a0
                P.tag = "A1.dsa.pv"
                for pp in range(4):
                    for (PT, pn, h) in ((PTe[s], ("PTe", s), pp), (PTo[s], ("PTo", s), 4 + pp)):
                        yb_ = h // 4
                        k.mm(YD[yb_][:, (h % 4) * 65:(h % 4) * 65 + 65], PT[:, pp * 128:(pp + 1) * 128], va1[:, kt, :], False, False,
                             [pn], [B(6 + yb_)], skip=True)
                        ystart_d[yb_] = False

        def norm(which):
            Yb, y0, b0, ys, tagn = (YM, 512, 4, 0, "A1.moba.norm") if which == "m" else (YD, 0, 6, 1, "A1.dsa.norm")
            P.tag = tagn
            for yb_ in range(2):
                k.actf(ysb[ys][:, yb_ * 4:(yb_ + 1) * 4, :], Yb[yb_][:, 0:260].rearrange("p (h d) -> p h d", h=4), AF.Copy,
                       [B(b0 + yb_)], [("ysb", ys)])
            k.tt("pool", rec[ys][:], ysb[ys][:, :, 64:65], cm1[:], ALU.pow, [("ysb", ys)], [("rec", ys)])
            k.tt("pool", yt[:, y0:y0 + 512].rearrange("p (h d) -> p h d", h=8), ysb[ys][:, :, 0:64], rec[ys][:].to_broadcast([128, 8, 64]),
                 ALU.mult, [("ysb", ys), ("rec", ys)], [("y", 1 - ys)])

        if WARMUP > 0:
            P.tag = "A1.warm"
            for wq in range(WARMUP):
                k.mm(banks[wq % 2][:, :], ident4[:, wq % 4, :], ident4[:], True, True, [], [B(wq % 2)])
        P.tag = "A1.yzero"
        for yb_ in range(4):
            P.act(lambda e, yb_=yb_: e.memzero(banks[4 + yb_][:, 0:260]), [], [B(4 + yb_)])
        sets = []
        for n, st_ in enumerate(steps):
            s_ = cc_state[0] % 2
            cc_state[0] += 1
            sets.append(s_)
            S(st_, s_)
            if n >= 1:
                V(steps[n - 1], sets[n - 1])
                if n == n_moba:
                    norm("m")
        V(steps[-1], sets[-1])
        norm("d")
        if debug:
            k.dma("sp", "dbgy", dbg["y"][i * 128:(i + 1) * 128, :], yt[:], [("y", 0), ("y", 1)], [])
        P.tag = "A1.oproj"
        for kc in range(8):
            k.tr(bankb[5][:, kc * 128:(kc + 1) * 128], yt[:, kc * 128:(kc + 1) * 128], identb[:], [("y", kc // 4)], [B(5)])
        k.actf(yT[:], bankb[5][:, :].rearrange("p (c t) -> p c t", c=8), AF.Copy, [B(5)], ["yT"])
        for nb in range(2):
            for kc in range(8):
                k.mm(banks[4 + nb][:, :], yT[:, kc, :], w_out[:, kc, nb * 512:(nb + 1) * 512], kc == 0, kc == 7,
                     ["yT", "w_out"], [B(4 + nb)])
            k.actf(tmp[:, nb * 512:(nb + 1) * 512], banks[4 + nb][:, :], AF.Copy, [B(4 + nb)], [("tmp", nb)])
        k.tt("pool", h1t[b][:], tmp[:], xin[b][:], ALU.add, [("tmp", 0), ("tmp", 1), ("xin", b)], [("h1t", b)])
        k.dma("sp", "h1%d" % b, h1_d[i * 128:(i + 1) * 128, :], h1t[b][:], [("h1t", b)], [("h1_d", i)])

    def load_q(i):
        b = i % 2
        k.dma("sp", "q%d" % b, qin[b][:], qs_d[i * 128:(i + 1) * 128, :], [], [("qin", b)])

    def load_x(i):
        b = i % 2
        k.dma("sp", "x1%d" % b, xin[b][:], x_d[i * 128:(i + 1) * 128, :], [], [("xin", b)])

    load_q(0)
    for i in range(NT + 1):
        if i < NT:
            a1_front(i)
        if i + 1 < NT:
            load_q(i + 1)
        if i >= 1:
            load_x(i - 1)
            a1_back(i - 1)
    if debug:
        P.barrier()
        k.dma("sp", "dbg2", dbg["kaT_end"], kaT2[:], [], [])
    if stage < 3:
        P.emit(nc)
        nc._prog_names = P.names
        return nc, sb

    P.barrier()
    sb.reset(mark_glob)
    wg = sb.alloc("wg", [128, 8, DFF], BF16)
    wu = sb.alloc("wu", [128, 8, DFF], BF16)
    wd = sb.alloc("wd", [128, NFC, D], BF16)
    wpg = sb.alloc("wpg", [128, 8, D], BF16)
    wpp = sb.alloc("wpp", [128, 2, D], BF16)
    gF = sb.alloc("gF", [128, 8], F32)
    gP = sb.alloc("gP", [128, 8], F32)
    gfin = sb.alloc("gfin", [128, D], F32)
    hb = [sb.alloc("hb", [128, 2, D], F32) for _ in range(2)]
    pb = [sb.alloc("pb", [128, 2, DPLE], F32) for _ in range(2)]
    hn = sb.alloc("hn", [128, D], BF16)
    fT = sb.alloc("fT", [128, 8, 256], BF16)
    actT = sb.alloc("actT", [128, NFC, 256], BF16)
    sgt = [sb.alloc("sgt", [128, 256], F32) for _ in range(2)]
    gtt = [sb.alloc("gtt", [128, 256], F32) for _ in range(2)]
    pn = sb.alloc("pn", [128, 2, DPLE], BF16)
    pT = sb.alloc("pT", [128, 2, 256], BF16)
    sgp = sb.alloc("sgp", [128, 512], F32)
    tpp = sb.alloc("tpp", [128, 512], F32)
    stB = [sb.alloc("stB", [128, 32], F32) for _ in range(2)]

    k.dma("sp", "gF", gF[:], gF_d, [], ["gF"])
    k.dma("sp", "gP", gP[:], gP_d, [], ["gP"])
    k.dma("sp", "gfin", gfin[:], gfin_d, [], ["gfin"])
    HB = DFF // 2
    for blk in range(2):
        cs_ = slice(blk * HB, (blk + 1) * HB)
        k.dma("pool", "wg%d" % blk, wg[:, :, cs_], w_gate_d[:, cs_].rearrange("(c p) n -> p c n", p=128), [], [("wg", blk)])
        k.dma("pool", "wu%d" % blk, wu[:, :, cs_], w_up_d[:, cs_].rearrange("(c p) n -> p c n", p=128), [], [("wu", blk)])
    for blk in range(2):
        k.dma("pool", "wd%d" % blk, wd[:, blk * 11:(blk + 1) * 11, :],
              w_down_d[blk * 1408:(blk + 1) * 1408, :].rearrange("(c p) n -> p c n", p=128), [], [("wd", blk)])
    k.dma("pool", "wpg", wpg[:], w_pg_d.rearrange("(c p) n -> p c n", p=128), [], ["wpg"])
    k.dma("pool", "wpp", wpp[:], w_pp_d.rearrange("(c p) n -> p c n", p=128), [], ["wpp"])

    NTB = NT // 2

    def b_loads(i):
        b = i % 2
        r0 = i * 256
        k.dma("sp", "hb%d" % b, hb[b][:], h1_d[r0:r0 + 256, :].rearrange("(s p) d -> p s d", p=128), [("h1_d", 2 * i), ("h1_d", 2 * i + 1)],
              [("hb", b, 0), ("hb", b, 1)])
        k.dma("sp", "pb%d" % b, pb[b][:], p_d[r0:r0 + 256, :].rearrange("(s p) d -> p s d", p=128), [], [("pb", b)])

    def norm_T(H, b, sub, S, c0, gvec, gname, dstT):
        hr = ("hb", b, sub)
        sr = lambda c: ("stB", b, c0 + c)
        k.actf(hn[:], H[:, sub, :], AF.Square, [hr], ["hn", sr(0)], accum=S[:, c0:c0 + 1])
        k.ts("dve", S[:, c0 + 1:c0 + 2], S[:, c0:c0 + 1], 1.0 / D, EPS, ALU.mult, ALU.add, [sr(0)], [sr(1)])
        k.tt("pool", S[:, c0 + 2:c0 + 3], S[:, c0 + 1:c0 + 2], cmh[:], ALU.pow, [sr(1)], [sr(2)])
        k.actf(hn[:], H[:, sub, :], AF.Copy, [hr, sr(2)], ["hn"], scale=S[:, c0 + 2:c0 + 3])
        for kc in range(8):
            k.tr(bankb[0][:, kc * 128:(kc + 1) * 128], hn[:, kc * 128:(kc + 1) * 128], identb[:], ["hn"], [B(0)])
        k.tt("dve", dstT[:, :, sub * 128:(sub + 1) * 128], bankb[0][:, :].rearrange("p (c t) -> p c t", c=8),
             gvec[:].unsqueeze(2).to_broadcast([128, 8, 128]), ALU.mult, [B(0), gname], [("fT", sub)])

    b_loads(0)
    for i in range(NTB):
        b = i % 2
        H = hb[b]
        S = stB[b]
        if i + 1 < NTB:
            b_loads(i + 1)
        P.tag = "B.norm"
        for sub in range(2):
            norm_T(H, b, sub, S, sub * 4, gF, "gF", fT)
        P.tag = "B.gu"
        for fc in range(NFC):
            s2 = fc % 2
            G, U = banks[2 + 2 * s2], banks[3 + 2 * s2]
            fs = slice(fc * 128, (fc + 1) * 128)
            for kc in range(8):
                k.mm(G[:, 0:256], wg[:, kc, fs], fT[:, kc, :], kc == 0, kc == 7, [("wg", fc // 11), ("fT", 0), ("fT", 1)], [B(2 + 2 * s2)])
            for kc in range(8):
                k.mm(U[:, 0:256], wu[:, kc, fs], fT[:, kc, :], kc == 0, kc == 7, [("wu", fc // 11), ("fT", 0), ("fT", 1)], [B(3 + 2 * s2)])
            k.actf(sgt[s2][:], G[:, 0:256], AF.Sigmoid, [B(2 + 2 * s2)], [("sgt", s2)])
            k.tt("dve", gtt[s2][:], G[:, 0:256], sgt[s2][:], ALU.mult, [B(2 + 2 * s2), ("sgt", s2)], [("gtt", s2)])
            k.tt("dve", actT[:, fc, :], gtt[s2][:], U[:, 0:256], ALU.mult, [B(3 + 2 * s2), ("gtt", s2)], [("actT", fc)])
        P.tag = "B.down"
        for sub in range(2):
            for nb in range(2):
                bk = 6 + (sub * 2 + nb) % 2
                for fc in range(NFC):
                    k.mm(banks[bk][:, :], actT[:, fc, sub * 128:(sub + 1) * 128], wd[:, fc, nb * 512:(nb + 1) * 512], fc == 0, fc == NFC - 1,
                         [("actT", fc), ("wd", fc // 11)], [B(bk)])
                hs = H[:, sub, nb * 512:(nb + 1) * 512]
                k.tt("dve", hs, banks[bk][:, :], hs, ALU.add, [B(bk), ("hb", b, sub)], [("hb", b, sub)])
        P.tag = "B.ple"
        k.cp("pool", pn[:], pb[b][:], [("pb", b)], ["pn"])
        for sub in range(2):
            for c in range(2):
                k.tr(bankb[1][:, (sub * 2 + c) * 128:(sub * 2 + c + 1) * 128], pn[:, sub, c * 128:(c + 1) * 128], identb[:], ["pn"], [B(1)])
        k.actf(pT[:].rearrange("p c (s t) -> p s c t", s=2), bankb[1][:, 0:512].rearrange("p (s c t) -> p s c t", s=2, c=2), AF.Copy,
               [B(1)], ["pT"])
        for sub in range(2):
            norm_T(H, b, sub, S, 8 + sub * 4, gP, "gP", fT)
        for sub in range(2):
            for nb in range(2):
                s2 = (sub * 2 + nb) % 2
                PG, PP = banks[2 + 2 * s2], banks[3 + 2 * s2]
                ns = slice(nb * 512, (nb + 1) * 512)
                for kc in range(8):
                    k.mm(PG[:, :], fT[:, kc, sub * 128:(sub + 1) * 128], wpg[:, kc, ns], kc == 0, kc == 7, [("fT", sub), "wpg"],
                         [B(2 + 2 * s2)])
                for c in range(2):
                    k.mm(PP[:, :], pT[:, c, sub * 128:(sub + 1) * 128], wpp[:, c, ns], c == 0, c == 1, ["pT", "wpp"], [B(3 + 2 * s2)])
                k.actf(sgp[:], PG[:, :], AF.Sigmoid, [B(2 + 2 * s2)], ["sgp"])
                k.tt("dve", tpp[:], sgp[:], PP[:, :], ALU.mult, ["sgp", B(3 + 2 * s2)], ["tpp"])
                hs = H[:, sub, ns]
                k.tt("pool", hs, hs, tpp[:], ALU.add, ["tpp", ("hb", b, sub)], [("hb", b, sub)])
        P.tag = "B.fin"
        for sub in range(2):
            c0 = 16 + sub * 4
            hr = ("hb", b, sub)
            sr = lambda c: ("stB", b, c0 + c)
            k.actf(hn[:], H[:, sub, :], AF.Square, [hr], ["hn", sr(0)], accum=S[:, c0:c0 + 1])
            k.ts("dve", S[:, c0 + 1:c0 + 2], S[:, c0:c0 + 1], 1.0 / D, EPS, ALU.mult, ALU.add, [sr(0)], [sr(1)])
            k.tt("pool", S[:, c0 + 2:c0 + 3], S[:, c0 + 1:c0 + 2], cmh[:], ALU.pow, [sr(1)], [sr(2)])
            k.stt(H[:, sub, :], H[:, sub, :], S[:, c0 + 2:c0 + 3], gfin[:], ALU.mult, ALU.mult, [hr, sr(2), "gfin"], [hr])
        k.dma("sp", "out%d" % b, out_d[i * 256:(i + 1) * 256, :].rearrange("(s p) d -> p s d", p=128), H[:],
              [("hb", b, 0), ("hb", b, 1)], [("out_d", i)])
    P.emit(nc)
    nc._prog_names = P.names
    return nc, sb


def _rope_table(L):
    half = 32
    inv = (np.float32(10000.0) ** (-(np.arange(half, dtype=np.float32) / np.float32(half)))).astype(np.float32)
    ang = np.arange(L, dtype=np.float32)[:, None] * inv[None, :]
    return np.concatenate([np.cos(ang), np.sin(ang)], axis=1).astype(np.float32)


def _cpar():
    kk = np.arange(128)
    return np.stack([(((kk % 16) % 2) == (c // 2)) for c in range(4)], axis=1).astype(np.float32)


def _csel2():
    kk = np.arange(128)[:, None]
    col = np.arange(64)[None, :]
    return (((kk // 16) == (col // 8)) & (((kk % 16) // 2) == (col % 8))).astype(np.float32)


def prep_inputs(inp, L):
    f = lambda a: np.ascontiguousarray(np.asarray(a, dtype=np.float32))
    g8 = lambda g: f(np.asarray(g).reshape(8, 128).T)
    shared = {
        "cs": _rope_table(L),
        "gA": g8(inp["g_attn"][0]), "gF": g8(inp["g_ffn"][0]), "gP": g8(inp["g_ple"][0]),
        "gfin": f(np.broadcast_to(np.asarray(inp["g_final"]).reshape(1, D), (128, D))),
        "gk": f(np.broadcast_to(np.asarray(inp["g_idx_k"][0]).reshape(1, 64), (128, 64))),
        "cpar": _cpar(), "csel2": _csel2(),
        "w_in": f(inp["w_in"][0]), "w_out": f(inp["w_out"][0]), "w_gate": f(inp["w_gate"][0]), "w_up": f(inp["w_up"][0]),
        "w_down": f(inp["w_down"][0]), "w_pg": f(inp["w_ple_gate"][0]), "w_pp": f(inp["w_ple_proj"][0]),
    }
    x = np.asarray(inp["x"], dtype=np.float32)
    p = np.asarray(inp["p"], dtype=np.float32)[0]
    return {"shared": shared, "x": [f(x[b]) for b in range(x.shape[0])], "p": [f(p[b]) for b in range(p.shape[0])]}


_NC_CACHE = {}


def kernel(**inputs):
    x = np.asarray(inputs["x"])
    Bn, L, _ = x.shape
    NT = L // 128
    prep = prep_inputs(inputs, L)
    if NT not in _NC_CACHE:
        _NC_CACHE[NT] = build(NT)[0]
    nc = _NC_CACHE[NT]
    in_maps = [dict(prep["shared"], x=prep["x"][b], p=prep["p"][b]) for b in range(Bn)]
    res = run_bass_kernel_spmd(nc, in_maps, core_ids=list(range(Bn)))
    return np.stack([np.asarray(res.results[b]["out"], dtype=np.float32) for b in range(Bn)], axis=0)
```
